# Optimizing a Trainium2 kernel written in Bass

```python
import math
import jax, jax.numpy as jnp
from jax import lax
import numpy as np

D_MODEL = 2048
BATCH = 2
SEQ = 16384
DEPTH = 1

CHUNK = 64
NORM_EPS = 1e-6

RWKV_HEADS = 16
RWKV_HEAD_DIM = 64
RWKV_WIDTH = RWKV_HEADS * RWKV_HEAD_DIM
DECAY_LORA = 96
AAA_LORA = 96
GATE_LORA = 256
RWKV_GN_EPS = 64e-5

DIFF_HEADS = 8
DIFF_QK_DIM = 64
DIFF_V_DIM = 2 * DIFF_QK_DIM
DIFF_QK_WIDTH = DIFF_HEADS * 2 * DIFF_QK_DIM
DIFF_V_WIDTH = DIFF_HEADS * DIFF_V_DIM
ROPE_THETA = 500000.0
ROPE_DIM = DIFF_QK_DIM // 4
Q_BLOCK = 128

N_EXPERTS = 32
TOP_K = 4
D_FF = D_MODEL
SWIGLU_LIMIT = 7.0
SWIGLU_ALPHA = 1.702
MOE_BLOCK = 128

SHIFT_SIZES = (RWKV_WIDTH, RWKV_WIDTH, RWKV_WIDTH, DECAY_LORA, AAA_LORA, GATE_LORA)
REST_SIZES = (DIFF_QK_WIDTH, DIFF_QK_WIDTH, DIFF_V_WIDTH, D_MODEL, D_MODEL)
SHIFT_WIDTH = 3 * RWKV_WIDTH + DECAY_LORA + AAA_LORA + GATE_LORA
IN_WIDTH = SHIFT_WIDTH + 2 * DIFF_QK_WIDTH + DIFF_V_WIDTH + 2 * D_MODEL

kernel_name = 'hybrid_rwkv7_diffattn_moe_block'


def _split(t, sizes):
    outs, start = [], 0
    for s in sizes:
        outs.append(t[..., start:start + s])
        start += s
    return outs


def rms_norm(x, gain, eps=NORM_EPS):
    xf = x.astype(jnp.float32)
    y = xf * lax.rsqrt(jnp.mean(xf * xf, axis=-1, keepdims=True) + eps)
    return (y * gain.astype(jnp.float32)).astype(x.dtype)


def token_shift(p, mu):
    prev = jnp.pad(p, ((0, 0), (1, 0), (0, 0)))[:, :-1]
    return p + (prev - p) * mu


def partial_rope(x, pos):
    half = ROPE_DIM // 2
    inv_freq = ROPE_THETA ** (-jnp.arange(half, dtype=jnp.float32) * 2.0 / ROPE_DIM)
    ang = pos.astype(jnp.float32)[:, None] * inv_freq[None, :]
    cos = jnp.cos(ang)[None, :, None, None, :]
    sin = jnp.sin(ang)[None, :, None, None, :]
    xr = x[..., :ROPE_DIM].astype(jnp.float32)
    x1, x2 = xr[..., :half], xr[..., half:]
    rot = jnp.concatenate([x1 * cos - x2 * sin, x2 * cos + x1 * sin], axis=-1)
    return jnp.concatenate([rot.astype(x.dtype), x[..., ROPE_DIM:]], axis=-1)


def rwkv7_branch(r, k, v, w_d, a_d, g_d, w0, w_decay_up, a0, w_aaa_up, w_gate_up,
                 k_k, k_a, r_k, gn_w, gn_b):
    B, S, _ = r.shape
    H, N = RWKV_HEADS, RWKV_HEAD_DIM
    f32 = jnp.float32
    w_log = -jax.nn.softplus(-(w0 + jnp.tanh(w_d) @ w_decay_up)) - 0.5
    a = jax.nn.sigmoid(a0 + a_d @ w_aaa_up)
    g = jax.nn.sigmoid(g_d) @ w_gate_up
    kk = (k * k_k).reshape(B, S, H, N).astype(f32)
    kk = kk / jnp.maximum(jnp.sqrt(jnp.sum(kk * kk, axis=-1, keepdims=True)), 1e-12)
    k = k * (1.0 + (a - 1.0) * k_a)

    def heads(t):
        return t.reshape(B, S, H, N).astype(f32)

    rh, kh, vh, ah = heads(r), heads(k), heads(v), heads(a)
    decay = jnp.exp(-jnp.exp(heads(w_log)))
    a_vec = -kk
    b_vec = kk * ah

    def step(state, inp):
        r_t, d_t, k_t, v_t, a_t, b_t = inp
        sa = jnp.einsum('bhvk,bhk->bhv', state, a_t)
        state = (state * d_t[:, :, None, :] + sa[..., None] * b_t[:, :, None, :]
                 + v_t[..., None] * k_t[:, :, None, :])
        y = jnp.einsum('bhvk,bhk->bhv', state, r_t)
        return state, y

    xs = tuple(jnp.moveaxis(t, 1, 0) for t in (rh, decay, kh, vh, a_vec, b_vec))
    _, ys = lax.scan(step, jnp.zeros((B, H, N, N), f32), xs)
    y = jnp.moveaxis(ys, 0, 1)
    mu = jnp.mean(y, axis=-1, keepdims=True)
    var = jnp.mean(jnp.square(y - mu), axis=-1, keepdims=True)
    yn = (y - mu) * lax.rsqrt(var + RWKV_GN_EPS)
    yn = yn * gn_w.reshape(H, N).astype(f32) + gn_b.reshape(H, N).astype(f32)
    bonus = jnp.sum(rh * kh * r_k.reshape(H, N).astype(f32), axis=-1, keepdims=True) * vh
    out = (yn + bonus).reshape(B, S, H * N).astype(r.dtype) * g
    return out


def diff_attention(q, k, v, q_gain, k_gain, lam_q1, lam_k1, lam_q2, lam_k2, subln_gain,
                   lambda_init):
    B, S, _ = q.shape
    H, Dh, Dv = DIFF_HEADS, DIFF_QK_DIM, DIFF_V_DIM
    f32 = jnp.float32
    q = rms_norm(q.reshape(B, S, H, 2, Dh), q_gain)
    k = rms_norm(k.reshape(B, S, H, 2, Dh), k_gain)
    v = v.reshape(B, S, H, Dv)
    pos = jnp.arange(S, dtype=jnp.int32)
    q = partial_rope(q, pos)
    k = partial_rope(k, pos)
    lam = (jnp.exp(jnp.sum(lam_q1.astype(f32) * lam_k1.astype(f32)))
           - jnp.exp(jnp.sum(lam_q2.astype(f32) * lam_k2.astype(f32))) + lambda_init)
    scale = Dh ** -0.5
    n_blocks = S // Q_BLOCK
    qb = q.reshape(B, n_blocks, Q_BLOCK, H, 2, Dh).transpose(1, 0, 2, 3, 4, 5)
    key_chunk = pos // CHUNK

    def attend(args):
        q_blk, blk = args
        q_chunk = (blk * Q_BLOCK + jnp.arange(Q_BLOCK, dtype=jnp.int32)) // CHUNK
        s = jnp.einsum('bqhmd,bkhmd->bhmqk', q_blk, k, preferred_element_type=f32) * scale
        mask = key_chunk[None, :] <= q_chunk[:, None]
        s = jnp.where(mask, s, -jnp.inf)
        p = jax.nn.softmax(s, axis=-1)
        attn = p[:, :, 0] - lam * p[:, :, 1]
        return jnp.einsum('bhqk,bkhd->bqhd', attn.astype(v.dtype), v)

    o = lax.map(attend, (qb, jnp.arange(n_blocks, dtype=jnp.int32)))
    o = o.transpose(1, 0, 2, 3, 4).reshape(B, S, H, Dv)
    o = rms_norm(o, subln_gain) * (1.0 - lambda_init)
    return o.reshape(B, S, H * Dv)


def moe_ffn(h, w_router, b_router, w_gate, b_gate, w_up, b_up, w_down, b_down):
    B, S, D = h.shape
    T = B * S
    M = T * TOP_K
    n_blocks = (M + N_EXPERTS * (MOE_BLOCK - 1) + MOE_BLOCK - 1) // MOE_BLOCK
    P = n_blocks * MOE_BLOCK
    hf = h.reshape(T, D)
    logits = (hf @ w_router + b_router).astype(jnp.float32)
    top_val, top_idx = lax.top_k(logits, TOP_K)
    top_w = jax.nn.softmax(top_val, axis=-1)

    flat_e = top_idx.reshape(M).astype(jnp.int32)
    order = jnp.argsort(flat_e)
    sorted_e = flat_e[order]
    counts = jnp.bincount(flat_e, length=N_EXPERTS)
    padded = (counts + MOE_BLOCK - 1) // MOE_BLOCK * MOE_BLOCK
    pad_end = jnp.cumsum(padded)
    pad_start = pad_end - padded
    sort_start = jnp.cumsum(counts) - counts
    dest = pad_start[sorted_e] + jnp.arange(M, dtype=jnp.int32) - sort_start[sorted_e]
    row_token = jnp.full((P,), T, jnp.int32).at[dest].set((order // TOP_K).astype(jnp.int32))
    row_gate = jnp.zeros((P,), jnp.float32).at[dest].set(top_w.reshape(M)[order])
    block_start = jnp.arange(n_blocks, dtype=jnp.int32) * MOE_BLOCK
    block_expert = jnp.minimum(jnp.searchsorted(pad_end, block_start, side='right'),
                               N_EXPERTS - 1).astype(jnp.int32)
    h_pad = jnp.concatenate([hf, jnp.zeros((1, D), hf.dtype)], axis=0)

    def expert_block(acc, args):
        tok, gw, e = args
        xb = h_pad[tok]
        gt = jnp.minimum(xb @ w_gate[e] + b_gate[e], SWIGLU_LIMIT)
        up = jnp.clip(xb @ w_up[e] + b_up[e], -SWIGLU_LIMIT, SWIGLU_LIMIT)
        y = ((up + 1.0) * gt * jax.nn.sigmoid(SWIGLU_ALPHA * gt)) @ w_down[e] + b_down[e]
        acc = acc.at[tok].add(y * gw[:, None].astype(y.dtype))
        return acc, None

    acc, _ = lax.scan(expert_block, jnp.zeros((T + 1, D), h.dtype),
                      (row_token.reshape(n_blocks, MOE_BLOCK),
                       row_gate.reshape(n_blocks, MOE_BLOCK), block_expert))
    return acc[:T].reshape(B, S, D)


def setup_inputs(seed: int = 0) -> dict:
    key = jax.random.key(seed)
    ks = iter(jax.random.split(key, 48))
    L, D, E, F = DEPTH, D_MODEL, N_EXPERTS, D_FF

    def nrm(shape, scale):
        return scale * jax.random.normal(next(ks), shape, jnp.float32)

    def gain(shape):
        return 1.0 + nrm(shape, 0.02)

    def unif(shape, lo, hi):
        return jax.random.uniform(next(ks), shape, jnp.float32, lo, hi)

    return {
        'x': nrm((BATCH, SEQ, D), 1.0),
        'c': nrm((BATCH, D), 1.0),
        'w_ada': nrm((L, D, 6 * D), 0.5 * D ** -0.5),
        'b_ada': nrm((L, 6 * D), 0.02),
        'norm1_gain': gain((L, D)),
        'norm2_gain': gain((L, D)),
        'w_in': nrm((L, D, IN_WIDTH), D ** -0.5),
        'shift_mu': unif((L, SHIFT_WIDTH), 0.0, 1.0),
        'w0': unif((L, RWKV_WIDTH), -5.0, 0.0),
        'w_decay_up': nrm((L, DECAY_LORA, RWKV_WIDTH), 0.1 * DECAY_LORA ** -0.5),
        'a0': nrm((L, RWKV_WIDTH), 0.1),
        'w_aaa_up': nrm((L, AAA_LORA, RWKV_WIDTH), 0.5 * AAA_LORA ** -0.5),
        'w_gate_up': nrm((L, GATE_LORA, RWKV_WIDTH), GATE_LORA ** -0.5),
        'k_k': 0.85 + nrm((L, RWKV_WIDTH), 0.02),
        'k_a': gain((L, RWKV_WIDTH)),
        'r_k': nrm((L, RWKV_WIDTH), 0.1),
        'gn_w': gain((L, RWKV_WIDTH)),
        'gn_b': nrm((L, RWKV_WIDTH), 0.02),
        'q_gain': gain((L, DIFF_QK_DIM)),
        'k_gain': gain((L, DIFF_QK_DIM)),
        'lam_q1': nrm((L, DIFF_QK_DIM), 0.1),
        'lam_k1': nrm((L, DIFF_QK_DIM), 0.1),
        'lam_q2': nrm((L, DIFF_QK_DIM), 0.1),
        'lam_k2': nrm((L, DIFF_QK_DIM), 0.1),
        'subln_gain': gain((L, DIFF_V_DIM)),
        'w_branch_a': nrm((L, RWKV_WIDTH, D), RWKV_WIDTH ** -0.5),
        'w_branch_b': nrm((L, DIFF_V_WIDTH, D), DIFF_V_WIDTH ** -0.5),
        'w_out': nrm((L, D, D), D ** -0.5),
        'w_router': nrm((L, D, E), D ** -0.5),
        'b_router': nrm((L, E), 0.01),
        'w_exp_gate': nrm((L, E, D, F), D ** -0.5),
        'b_exp_gate': nrm((L, E, F), 0.01),
        'w_exp_up': nrm((L, E, D, F), D ** -0.5),
        'b_exp_up': nrm((L, E, F), 0.01),
        'w_exp_down': nrm((L, E, F, D), F ** -0.5),
        'b_exp_down': nrm((L, E, D), 0.01),
    }


def reference(x, c, w_ada, b_ada, norm1_gain, norm2_gain, w_in, shift_mu, w0, w_decay_up,
              a0, w_aaa_up, w_gate_up, k_k, k_a, r_k, gn_w, gn_b, q_gain, k_gain,
              lam_q1, lam_k1, lam_q2, lam_k2, subln_gain, w_branch_a, w_branch_b, w_out,
              w_router, b_router, w_exp_gate, b_exp_gate, w_exp_up, b_exp_up,
              w_exp_down, b_exp_down):
    for l in range(DEPTH):
        mod = jax.nn.silu(c) @ w_ada[l] + b_ada[l]
        sh1, sc1, g1, sh2, sc2, g2 = jnp.split(mod[:, None, :], 6, axis=-1)

        h = rms_norm(x, norm1_gain[l]) * (1.0 + sc1) + sh1
        proj = h @ w_in[l]
        shifted = token_shift(proj[..., :SHIFT_WIDTH], shift_mu[l])
        r, k, v, w_d, a_d, g_d = _split(shifted, SHIFT_SIZES)
        qd, kd, vd, gate_a, gate_b = _split(proj[..., SHIFT_WIDTH:], REST_SIZES)

        o_a = rwkv7_branch(r, k, v, w_d, a_d, g_d, w0[l], w_decay_up[l], a0[l], w_aaa_up[l],
                           w_gate_up[l], k_k[l], k_a[l], r_k[l], gn_w[l], gn_b[l])
        lambda_init = 0.8 - 0.6 * math.exp(-0.3 * l)
        o_b = diff_attention(qd, kd, vd, q_gain[l], k_gain[l], lam_q1[l], lam_k1[l],
                             lam_q2[l], lam_k2[l], subln_gain[l], lambda_init)

        merged = (jax.nn.sigmoid(gate_a) * (o_a @ w_branch_a[l])
                  + jax.nn.sigmoid(gate_b) * (o_b @ w_branch_b[l]))
        x = x + g1 * (merged @ w_out[l])

        h2 = rms_norm(x, norm2_gain[l]) * (1.0 + sc2) + sh2
        x = x + g2 * moe_ffn(h2, w_router[l], b_router[l], w_exp_gate[l], b_exp_gate[l],
                             w_exp_up[l], b_exp_up[l], w_exp_down[l], b_exp_down[l])
    return x
```

```python
import numpy as np
import ml_dtypes
from contextlib import ExitStack
import concourse.bass as bass
import concourse.mybir as mybir
from concourse.bass_utils import run_bass_kernel_spmd

F32 = mybir.dt.float32
BF16 = mybir.dt.bfloat16
I32 = mybir.dt.int32
AF = mybir.ActivationFunctionType
ALU = mybir.AluOpType
AX = mybir.AxisListType

D = 2048
NCORES = 8


class Buf:
    _n = 0

    def __init__(self, name=""):
        Buf._n += 1
        self.name = f"{name}#{Buf._n}"
        self.w = None
        self.r = []


class KB:
    def __init__(self, nc, n_dma_sems=24):
        self.nc = nc
        self.eng = {"pe": nc.tensor, "act": nc.scalar, "dve": nc.vector, "pool": nc.gpsimd, "sp": nc.sync}
        self.sem = {e: nc.alloc_semaphore(name=f"es_{e}") for e in self.eng}
        self.cnt = {e: 0 for e in self.eng}
        self.seen = {e: {} for e in self.eng}
        self.dsem = [nc.alloc_semaphore(name=f"ds_{i}") for i in range(n_dma_sems)]
        self.dgen = [0] * n_dma_sems
        self.dnext = 0

    def _wait(self, e, tok):
        if tok is None:
            return
        key, val = tok
        if self.seen[e].get(key, 0) >= val:
            return
        self.seen[e][key] = val
        sem = self.sem[key] if isinstance(key, str) else self.dsem[key]
        self.eng[e].wait_ge(sem, val)

    def _deps(self, e, reads, writes):
        for b in reads:
            self._wait(e, b.w)
        for b in writes:
            self._wait(e, b.w)
            for t in b.r:
                self._wait(e, t)

    def _mark(self, tok, reads, writes):
        for b in reads:
            b.r.append(tok)
            if len(b.r) > 64:
                b.r = b.r[-64:]
        for b in writes:
            b.w = tok
            b.r = []

    def op(self, e, fn, reads=(), writes=()):
        self._deps(e, reads, writes)
        ins = fn(self.eng[e])
        self.cnt[e] += 1
        ins.then_inc(self.sem[e], 1)
        tok = (e, self.cnt[e])
        self._mark(tok, reads, writes)
        return tok

    def dma(self, e, out, in_, reads=(), writes=(), **kw):
        j = self.dnext
        self.dnext = (self.dnext + 1) % len(self.dsem)
        self._deps(e, reads, writes)
        if self.dgen[j] > 0:
            self._wait(e, (j, 16 * self.dgen[j]))
        self.dgen[j] += 1
        self.eng[e].dma_start(out=out, in_=in_, **kw).then_inc(self.dsem[j], 16)
        tok = (j, 16 * self.dgen[j])
        self._mark(tok, reads, writes)
        return tok

    def idma_gather(self, out, src_rows, idx, reads=(), writes=()):
        j = self.dnext
        self.dnext = (self.dnext + 1) % len(self.dsem)
        self._deps("pool", reads, writes)
        if self.dgen[j] > 0:
            self._wait("pool", (j, 16 * self.dgen[j]))
        self.dgen[j] += 1
        self.nc.gpsimd.indirect_dma_start(out=out, out_offset=None, in_=src_rows,
                                          in_offset=bass.IndirectOffsetOnAxis(ap=idx, axis=0)).then_inc(self.dsem[j], 16)
        tok = (j, 16 * self.dgen[j])
        self._mark(tok, reads, writes)
        return tok

    def idma_scatter(self, dst_rows, idx, src, reads=(), writes=()):
        j = self.dnext
        self.dnext = (self.dnext + 1) % len(self.dsem)
        self._deps("pool", reads, writes)
        if self.dgen[j] > 0:
            self._wait("pool", (j, 16 * self.dgen[j]))
        self.dgen[j] += 1
        self.nc.gpsimd.indirect_dma_start(out=dst_rows, out_offset=bass.IndirectOffsetOnAxis(ap=idx, axis=0),
                                          in_=src, in_offset=None).then_inc(self.dsem[j], 16)
        tok = (j, 16 * self.dgen[j])
        self._mark(tok, reads, writes)
        return tok

    def collective(self, kind, groups, in_t, out_ap, reads=(), writes=()):
        if not hasattr(self, "csem"):
            self.csem = self.nc.alloc_semaphore(name="cc_sem")
            self.ccnt = 0
            self.sem["cc"] = self.csem
        self._deps("pool", reads, writes)
        self.ccnt += 1
        self.nc.gpsimd.collective_compute(kind, ALU.bypass, replica_groups=groups, ins=[in_t], outs=[out_ap]).then_inc(self.csem)
        tok = ("cc", self.ccnt)
        self._mark(tok, reads, writes)
        return tok

    def barrier(self):
        for e in self.eng:
            for e2 in self.eng:
                if e2 != e and self.cnt[e2] > 0:
                    self._wait(e, (e2, self.cnt[e2]))
            for j, g in enumerate(self.dgen):
                if g > 0:
                    self._wait(e, (j, 16 * g))
            if getattr(self, "ccnt", 0) > 0:
                self._wait(e, ("cc", self.ccnt))

    def drain(self, e, bufs):
        for b in bufs:
            self._wait(e, b.w)


def make_scope(nc, kb):
    class Scope:
        def __init__(self):
            self.es = ExitStack()
        def sb(self, name, shape, dt):
            Buf._n += 1
            return self.es.enter_context(nc.sbuf_tensor(f"{name}_{Buf._n}", shape, dt))
        def ps(self, name, shape, dt=F32):
            Buf._n += 1
            return self.es.enter_context(nc.psum_tensor(f"{name}_{Buf._n}", shape, dt))
        def close(self):
            kb.barrier()
            self.es.close()
    return Scope


def rwkv_const_inputs(nc):
    d = {}
    for nm, shp in (("vecs", [7, 256]), ("mu", [128, 10]), ("msk", [128, 256]), ("mls", [128, 128]), ("triu", [128, 128]),
                    ("trils", [128, 128]), ("ident", [128, 128]), ("wdu", [96, 256]), ("wau", [96, 256]), ("wgu", [256, 256])):
        d[nm] = nc.dram_tensor("r_" + nm, shp, F32, kind="ExternalInput").ap()
    return d


def rwkv_const_host():
    s_ = np.arange(128)[:, None]; t_ = np.arange(128)[None, :]
    return {
        "r_msk": np.concatenate([(s_ < t_), (s_ <= t_)], axis=1).astype(np.float32),
        "r_mls": (s_ > t_).astype(np.float32),
        "r_triu": (s_ <= t_).astype(np.float32),
        "r_trils": (s_ > t_).astype(np.float32),
        "r_ident": np.eye(128, dtype=np.float32),
    }


def build_rwkv_test(S, lim=999):
    nc = bass.Bass("TRN2", target_bir_lowering=False)
    projT = nc.dram_tensor("projT", [1984, S], F32, kind="ExternalInput").ap()
    o_a = nc.dram_tensor("o_a", [S, 256], F32, kind="ExternalOutput").ap()
    cst = rwkv_const_inputs(nc)
    kb = KB(nc)
    Scope = make_scope(nc, kb)
    outs = []
    phase_rwkv(nc, kb, Scope, None, projT, S, cst, o_a, outs, lim=lim)
    kb.drain("sp", outs)
    return nc


def build(S, stage=99):
    nc = bass.Bass("TRN2", target_bir_lowering=False)
    KC = D // 128
    NT = S // 128
    x = nc.dram_tensor("x", [S, D], F32, kind="ExternalInput").ap()
    c = nc.dram_tensor("c", [128, KC], F32, kind="ExternalInput").ap()
    w_ada = nc.dram_tensor("w_ada", [D, 6 * D], F32, kind="ExternalInput").ap()
    b_ada = nc.dram_tensor("b_ada", [128, 6 * KC], F32, kind="ExternalInput").ap()
    g1n = nc.dram_tensor("norm1_gain", [128, KC], F32, kind="ExternalInput").ap()
    NP1 = 1984
    w_in1 = nc.dram_tensor("w_in1", [D, NP1], F32, kind="ExternalInput").ap()
    projT = nc.dram_tensor("projT", [NP1, S], F32, kind="ExternalOutput").ap()
    modo = nc.dram_tensor("modo", [128, 6 * KC], F32, kind="ExternalOutput").ap()
    ident_in = nc.dram_tensor("ident_in", [128, 128], F32, kind="ExternalInput").ap()

    kb = KB(nc)
    outs = []

    Scope = make_scope(nc, kb)

    G = Scope()
    ident = G.sb("ident", [128, 128], BF16)
    identf = G.sb("identf", [128, 128], F32)
    b_ident = Buf("ident")
    kb.dma("sp", identf[:], ident_in[:, :], writes=[b_ident])
    kb.op("dve", lambda e: e.tensor_copy(out=ident[:], in_=identf[:]), reads=[b_ident], writes=[b_ident])
    mod = G.sb("mod", [128, 6 * KC], F32); b_mod = Buf("mod")
    hs = G.sb("hs", [128, KC], F32); b_hs = Buf("hs")

    A = Scope()
    c_sb = A.sb("c_sb", [128, KC], F32); b_c = Buf("c")
    sc_bf = A.sb("sc_bf", [128, KC], BF16)
    bada = A.sb("bada", [128, 6 * KC], F32); b_bada = Buf("bada")
    kb.dma("sp", c_sb[:], c[:, :], writes=[b_c])
    kb.dma("sp", bada[:], b_ada[:, :], writes=[b_bada])
    kb.op("act", lambda e: e.activation(out=sc_bf[:], in_=c_sb[:], func=AF.Silu), reads=[b_c], writes=[b_c])
    GW = 256
    wa_f = [A.sb(f"wa_f{i}", [128, KC, GW], F32) for i in range(2)]
    wa_b = [A.sb(f"wa_b{i}", [128, KC, GW], BF16) for i in range(2)]
    b_waf = [Buf("waf0"), Buf("waf1")]
    b_wab = [Buf("wab0"), Buf("wab1")]
    pmod = A.ps("pmod", [128, 8]); b_pmod = Buf("pmod")
    w_ada_v = w_ada.rearrange("(kc p) n -> p kc n", p=128)
    for g in range(6 * D // GW):
        i = g % 2
        kb.dma("sp" if i == 0 else "act", wa_f[i][:], w_ada_v[:, :, g * GW:(g + 1) * GW], writes=[b_waf[i]])
        kb.op("dve" if i == 0 else "pool", lambda e: e.tensor_copy(out=wa_b[i][:], in_=wa_f[i][:]),
              reads=[b_waf[i]], writes=[b_wab[i]])
        for nb in range(GW // 128):
            col = g * (GW // 128) + nb
            pc = (col % 4) * 2
            for k in range(KC):
                kb.op("pe", lambda e: e.matmul(pmod[:, pc:pc + 1],
                                               lhsT=wa_b[i][:, k, nb * 128:(nb + 1) * 128],
                                               rhs=sc_bf[:, k:k + 1], start=(k == 0), stop=(k == KC - 1)),
                      reads=[b_wab[i], b_c], writes=[b_pmod])
            kb.op("dve", lambda e: e.tensor_tensor(out=mod[:, col:col + 1], in0=pmod[:, pc:pc + 1],
                                                   in1=bada[:, col:col + 1], op=ALU.add),
                  reads=[b_pmod, b_bada], writes=[b_mod])
    b_modo = Buf("modo"); outs.append(b_modo)
    kb.dma("sp", modo[:, :], mod[:], reads=[b_mod], writes=[b_modo])
    g1 = A.sb("g1", [128, KC], F32); b_g1 = Buf("g1")
    kb.dma("sp", g1[:], g1n[:, :], writes=[b_g1])
    kb.op("dve", lambda e: e.scalar_tensor_tensor(out=hs[:], in0=mod[:, KC:2 * KC], scalar=1.0, in1=g1[:],
                                                  op0=ALU.add, op1=ALU.mult), reads=[b_mod, b_g1], writes=[b_hs])
    A.close()

    P = Scope()
    TG = min(1024, S)
    w_v = w_in1.rearrange("(kc p) n -> p kc n", p=128)
    Wb = P.sb("Wb", [128, KC, NP1], BF16); b_Wb = Buf("Wb")
    wf = [P.sb(f"wf{i}", [128, KC, 128], F32) for i in range(2)]; b_wf = [Buf("wf0"), Buf("wf1")]
    nblocks = [(n0, min(128, NP1 - n0)) for n0 in range(0, NP1, 128)]
    for bi, (n0, nw) in enumerate(nblocks):
        i = bi % 2
        kb.dma("act", wf[i][:, :, :nw], w_v[:, :, n0:n0 + nw], writes=[b_wf[i]])
        kb.op("pool", lambda e: e.tensor_copy(out=Wb[:, :, n0:n0 + nw], in_=wf[i][:, :, :nw]), reads=[b_wf[i]], writes=[b_Wb])
    hT = [P.sb(f"hT{i}", [128, KC, TG], BF16) for i in range(2)]; b_hT = [Buf("hT0"), Buf("hT1")]
    xt = [P.sb(f"xt{i}", [128, D], F32) for i in range(2)]; b_xt = [Buf("xt0"), Buf("xt1")]
    xs = [P.sb(f"xs{i}", [128, D], BF16) for i in range(2)]; b_xs = [Buf("xs0"), Buf("xs1")]
    junk = P.sb("junk", [128, D], BF16); b_junk = Buf("junk")
    ssq = [P.sb(f"ssq{i}", [128, 1], F32) for i in range(2)]; b_ssq = [Buf("ssq0"), Buf("ssq1")]
    ptr = [P.ps(f"ptr{i}", [128, 4, 128], BF16) for i in range(2)]; b_ptr = [Buf("ptr0"), Buf("ptr1")]
    pp = [P.ps(f"pp{i}", [128, 512]) for i in range(2)]; b_pp = [Buf("pp0"), Buf("pp1")]
    ot = [P.sb(f"ot{i}", [128, 512], F32) for i in range(2)]; b_ot = [Buf("ot0"), Buf("ot1")]
    TB = min(512, TG)
    cnt = 0
    tcount = 0
    for gi, tg0 in enumerate(range(0, S, TG)):
        hi = gi % 2
        for tt in range(TG // 128):
            t = tg0 // 128 + tt
            i = tcount % 2
            tcount += 1
            kb.dma("sp", xt[i][:], x[t * 128:(t + 1) * 128, :], writes=[b_xt[i]])
            kb.op("act", lambda e: e.activation(out=junk[:], in_=xt[i][:], func=AF.Square, accum_out=ssq[i][:]),
                  reads=[b_xt[i]], writes=[b_junk, b_ssq[i]])
            kb.op("act", lambda e: e.activation(out=ssq[i][:], in_=ssq[i][:], func=AF.Sqrt, scale=1.0 / D, bias=1e-6),
                  reads=[b_ssq[i]], writes=[b_ssq[i]])
            kb.op("dve", lambda e: e.reciprocal(out=ssq[i][:], in_=ssq[i][:]), reads=[b_ssq[i]], writes=[b_ssq[i]])
            kb.op("dve", lambda e: e.tensor_scalar(out=xs[i][:], in0=xt[i][:], scalar1=ssq[i][:, 0:1], scalar2=None,
                                                   op0=ALU.mult), reads=[b_xt[i], b_ssq[i]], writes=[b_xs[i]])
            for q in range(KC // 4):
                pi = q % 2
                for u in range(4):
                    k = q * 4 + u
                    kb.op("pe", lambda e: e.transpose(out=ptr[pi][:, u, :], in_=xs[i][:, k * 128:(k + 1) * 128],
                                                      identity=ident[:]),
                          reads=[b_xs[i], b_ident], writes=[b_ptr[pi]])
                for u in range(4):
                    k = q * 4 + u
                    kb.op("act", lambda e: e.activation(out=hT[hi][:, k, tt * 128:(tt + 1) * 128], in_=ptr[pi][:, u, :],
                                                        func=AF.Identity, scale=hs[:, k:k + 1], bias=mod[:, k:k + 1]),
                          reads=[b_ptr[pi], b_hs, b_mod], writes=[b_hT[hi]])
        for bi, (n0, nw) in enumerate(nblocks):
            for t0 in range(0, TG, TB):
                pi = cnt % 2
                cnt += 1
                for k in range(KC):
                    kb.op("pe", lambda e: e.matmul(pp[pi][:nw, :TB], lhsT=Wb[:, k, n0:n0 + nw], rhs=hT[hi][:, k, t0:t0 + TB],
                                                   start=(k == 0), stop=(k == KC - 1)),
                          reads=[b_Wb, b_hT[hi]], writes=[b_pp[pi]])
                kb.op("act" if pi == 0 else "dve",
                      (lambda e: e.activation(out=ot[pi][:nw, :TB], in_=pp[pi][:nw, :TB], func=AF.Identity)) if pi == 0 else
                      (lambda e: e.tensor_copy(out=ot[pi][:nw, :TB], in_=pp[pi][:nw, :TB])),
                      reads=[b_pp[pi]], writes=[b_ot[pi]])
                ob = Buf("o"); outs.append(ob)
                kb.dma("sp", projT[n0:n0 + nw, tg0 + t0:tg0 + t0 + TB], ot[pi][:nw, :TB], reads=[b_ot[pi]], writes=[ob])
    P.close()

    kb.drain("sp", outs)
    return nc


def _feat(v):
    return np.ascontiguousarray(v.reshape(-1, 128).T)


def core_cols(j):
    r = list(range(256 * j, 256 * (j + 1)))
    cols = r + [1024 + q for q in r] + [2048 + q for q in r] + list(range(3072, 3520))
    cols += [3520 + q for q in r] + [4544 + q for q in r] + [5568 + q for q in r]
    return np.array(cols)


class _Stop(Exception):
    pass


def phase_rwkv(nc, kb, Scope, G, projT, S, cst, o_a, outs, lim=999, fm_out=False):
    R = Scope()
    try:
        _phase_rwkv(nc, kb, R, G, projT, S, cst, o_a, outs, lim, fm_out)
    except _Stop:
        pass
    R.close()


def _phase_rwkv(nc, kb, R, G, projT, S, cst, o_a, outs, lim, fm_out=False):
    def chk(n):
        if n >= lim:
            raise _Stop()
    SC = min(512, S)
    NSC = S // SC
    CPS = SC // 128
    EH = float(np.exp(-0.5))
    b_cst = Buf("rcst")
    vecs = R.sb("vecs", [128, 7, 256], F32)
    kb.dma("sp", vecs[:], cst["vecs"].partition_broadcast(128), writes=[b_cst])
    mu = R.sb("mu", [128, 10], F32)
    kb.dma("sp", mu[:], cst["mu"], writes=[b_cst])
    msk = R.sb("msk", [128, 256], F32)
    mls = R.sb("mls", [128, 128], F32)
    triu = R.sb("triu", [128, 128], F32)
    trils = R.sb("trils", [128, 128], F32)
    identf = R.sb("r_identf", [128, 128], F32)
    identb = R.sb("r_identb", [128, 128], BF16)
    ones = R.sb("ones", [128, 1], F32)
    for tile, nm in ((msk, "msk"), (mls, "mls"), (triu, "triu"), (trils, "trils"), (identf, "ident")):
        kb.dma("sp", tile[:], cst[nm], writes=[b_cst])
    kb.op("dve", lambda e: e.tensor_copy(out=identb[:], in_=identf[:]), reads=[b_cst], writes=[b_cst])
    kb.op("dve", lambda e: e.memset(ones[:], 1.0), writes=[b_cst])
    lw_f = R.sb("lw_f", [128, 2, 256], F32)
    wg_f = R.sb("wg_f", [128, 2, 256], F32)
    wdu = R.sb("wdu", [128, 2, 256], BF16)
    wgu = R.sb("wgu", [128, 2, 256], BF16)
    kb.dma("sp", lw_f[:96, 0, :], cst["wdu"], writes=[b_cst])
    kb.dma("sp", lw_f[:96, 1, :], cst["wau"], writes=[b_cst])
    kb.dma("sp", wg_f[:], cst["wgu"].rearrange("(kc p) n -> p kc n", p=128), writes=[b_cst])
    kb.op("dve", lambda e: e.tensor_copy(out=wdu[:96], in_=lw_f[:96]), reads=[b_cst], writes=[b_cst])
    kb.op("dve", lambda e: e.tensor_copy(out=wgu[:], in_=wg_f[:]), reads=[b_cst], writes=[b_cst])

    chk(1)
    St = [R.sb(f"St{p}", [128, 64], F32) for p in range(2)]
    Sb = [R.sb(f"Sb{p}", [128, 64], BF16) for p in range(2)]
    b_S = [Buf("S0"), Buf("S1")]
    for p in range(2):
        kb.op("pool", lambda e: e.memset(St[p][:], 0.0), writes=[b_S[p]])
        kb.op("pool", lambda e: e.memset(Sb[p][:], 0.0), writes=[b_S[p]])

    blocks = [(0, 128), (128, 128), (256, 128), (384, 128), (512, 128), (640, 128), (768, 96), (864, 96), (960, 128), (1088, 128)]
    pt = [[R.sb(f"pt{i}_{q}", [128, SC + 1], F32) for q in range(10)] for i in range(2)]
    b_pt = [[Buf(f"pt{i}_{q}") for q in range(10)] for i in range(2)]
    st = [R.sb(f"st{q}", [128, SC], F32) for q in range(10)]
    b_st = [Buf(f"st{q}") for q in range(10)]
    tmpA = R.sb("tmpA", [128, SC], F32); b_tmpA = Buf("tmpA")

    NPS = 6
    pst = [R.ps(f"rps{i}", [128, 512], F32) for i in range(NPS)]
    b_pst = [Buf(f"rps{i}") for i in range(NPS)]
    ptbs = [R.ps(f"rptb{p}", [128, 4, 128], BF16) for p in range(2)]; b_ptbs = [Buf("rptb0"), Buf("rptb1")]
    pctr = [0]

    def PS():
        i = pctr[0] % NPS
        pctr[0] += 1
        return pst[i], b_pst[i]

    def T(name, shape, dt):
        return R.sb(name, shape, dt), Buf(name)
    twd, b_twd = T("twd", [128, 128], BF16)
    adb, b_adb = T("adb", [128, 128], BF16)
    sg, b_sg = T("sg", [128, 2, 128], BF16)
    lw, b_lw = T("lw", [128, 256], F32)
    asig, b_asig = T("asig", [128, 256], F32)
    gt, b_gt = T("gt", [128, 256], F32)
    r_tm, b_r = T("r_tm", [128, 256], F32)
    k_tm, b_k = T("k_tm", [128, 256], F32)
    v_tm, b_v = T("v_tm", [128, 256], F32)
    v_bf, b_vb = T("v_bf", [128, 256], BF16)
    kk, b_kk = T("kk", [128, 256], F32)
    km, b_km = T("km", [128, 256], F32)
    bv, b_bv = T("bv", [128, 256], F32)
    t1, b_t1 = T("t1", [128, 256], F32)
    t2, b_t2 = T("t2", [128, 256], F32)
    ss, b_ss = T("ss", [128, 4], F32)
    bonus, b_bonus = T("bonus", [128, 4], F32)
    e_pos, b_epos = T("e_pos", [128, 256], F32)
    e_neg, b_eneg = T("e_neg", [128, 256], F32)
    e_prev, b_eprev = T("e_prev", [128, 256], F32)
    e_end, b_eend = T("e_end", [128, 256], F32)
    rt_b, b_rtb = T("rt_b", [128, 256], BF16)
    at_b, b_atb = T("at_b", [128, 256], BF16)
    bt_b, b_btb = T("bt_b", [128, 256], BF16)
    kt_b, b_ktb = T("kt_b", [128, 256], BF16)
    bh_b, b_bhb = T("bh_b", [128, 256], BF16)
    kh_b, b_khb = T("kh_b", [128, 256], BF16)
    dC, b_dC = T("dC", [128, 2], F32)
    AR = [R.sb(f"AR{p}", [128, 2, 128], BF16) for p in range(2)]; b_AR = [Buf("AR0"), Buf("AR1")]
    KT = [R.sb(f"KT{p}", [128, 128], BF16) for p in range(2)]; b_KT = [Buf("KT0"), Buf("KT1")]
    BT = [R.sb(f"BT{p}", [128, 128], BF16) for p in range(2)]; b_BT = [Buf("BT0"), Buf("BT1")]
    HG2, b_HG2 = T("HG2", [128, 256], BF16)
    G1m, b_G1m = T("G1m", [128, 128], BF16)
    Nc = [R.sb(f"Nc{i}", [128, 128], F32) for i in range(2)]; b_Nc = [Buf("Nc0"), Buf("Nc1")]
    Lc = [R.sb(f"Lc{i}", [128, 128], F32) for i in range(2)]; b_Lc = [Buf("Lc0"), Buf("Lc1")]
    Pm = [R.sb(f"Pm{i}", [128, 128], F32) for i in range(2)]; b_Pm = [Buf("Pm0"), Buf("Pm1")]
    W0, b_W0 = T("W0", [128, 64], F32)
    Ub, b_Ub = T("Ub", [128, 256], BF16)
    y_tm, b_y = T("y_tm", [128, 256], F32)
    yc, b_yc = T("yc", [128, 256], F32)
    s1, b_s1 = T("s1", [128, 4], F32)
    s2, b_s2 = T("s2", [128, 4], F32)
    oa, b_oa = T("oa", [128, 256], F32)

    def v3(ap):
        return ap.rearrange("p (h n) -> p h n", h=4)

    def bc(ap4):
        return ap4.unsqueeze(2).to_broadcast([128, 4, 64])

    for sc in range(NSC):
        i = sc % 2
        t0 = sc * SC
        for q, (r0, nr) in enumerate(blocks):
            if sc == 0:
                kb.op("pool", lambda e: e.memset(pt[i][q][:nr, 0:1], 0.0), writes=[b_pt[i][q]])
                kb.dma("sp", pt[i][q][:nr, 1:SC + 1], projT[r0:r0 + nr, 0:SC], writes=[b_pt[i][q]])
            else:
                kb.dma("sp", pt[i][q][:nr, :], projT[r0:r0 + nr, t0 - 1:t0 + SC], writes=[b_pt[i][q]])
            eng = "pool" if q % 2 == 0 else "dve"
            kb.op(eng, lambda e: e.tensor_tensor(out=tmpA[:nr, :], in0=pt[i][q][:nr, 0:SC], in1=pt[i][q][:nr, 1:SC + 1],
                                                 op=ALU.subtract), reads=[b_pt[i][q]], writes=[b_tmpA])
            kb.op("dve", lambda e: e.scalar_tensor_tensor(out=st[q][:nr, :], in0=tmpA[:nr, :], scalar=mu[:nr, q:q + 1],
                                                        in1=pt[i][q][:nr, 1:SC + 1], op0=ALU.mult, op1=ALU.add),
                  reads=[b_tmpA, b_pt[i][q], b_cst], writes=[b_st[q]])
        chk(2)
        for ci in range(CPS):
            c0 = ci * 128
            cs = slice(c0, c0 + 128)
            kb.op("act", lambda e: e.activation(out=twd[:96, :], in_=st[6][:96, cs], func=AF.Tanh), reads=[b_st[6]], writes=[b_twd])
            kb.op("act", lambda e: e.activation(out=adb[:96, :], in_=st[7][:96, cs], func=AF.Identity), reads=[b_st[7]], writes=[b_adb])
            for kc in range(2):
                kb.op("act", lambda e: e.activation(out=sg[:, kc, :], in_=st[8 + kc][:, cs], func=AF.Sigmoid),
                      reads=[b_st[8 + kc]], writes=[b_sg])
            chk(3)
            p_, bp_ = PS()
            kb.op("pe", lambda e: e.matmul(p_[:, 0:256], lhsT=twd[:96, :], rhs=wdu[:96, 0, :], start=True, stop=True),
                  reads=[b_twd, b_cst], writes=[bp_])
            kb.op("dve", lambda e: e.tensor_tensor(out=t1[:], in0=p_[:, 0:256], in1=vecs[:, 0, :], op=ALU.add),
                  reads=[bp_, b_cst], writes=[b_t1])
            kb.op("act", lambda e: e.activation(out=t1[:], in_=t1[:], func=AF.Sigmoid), reads=[b_t1], writes=[b_t1])
            kb.op("dve", lambda e: e.tensor_scalar(out=lw[:], in0=t1[:], scalar1=-EH, scalar2=None, op0=ALU.mult),
                  reads=[b_t1], writes=[b_lw])
            chk(4)
            p_, bp_ = PS()
            kb.op("pe", lambda e: e.matmul(p_[:, 0:256], lhsT=adb[:96, :], rhs=wdu[:96, 1, :], start=True, stop=True),
                  reads=[b_adb, b_cst], writes=[bp_])
            kb.op("dve", lambda e: e.tensor_tensor(out=asig[:], in0=p_[:, 0:256], in1=vecs[:, 1, :], op=ALU.add),
                  reads=[bp_, b_cst], writes=[b_asig])
            kb.op("act", lambda e: e.activation(out=asig[:], in_=asig[:], func=AF.Sigmoid), reads=[b_asig], writes=[b_asig])
            chk(5)
            p_, bp_ = PS()
            for kc in range(2):
                kb.op("pe", lambda e: e.matmul(p_[:, 0:256], lhsT=sg[:, kc, :], rhs=wgu[:, kc, :], start=(kc == 0), stop=(kc == 1)),
                      reads=[b_sg, b_cst], writes=[bp_])
            kb.op("act", lambda e: e.activation(out=gt[:], in_=p_[:, 0:256], func=AF.Identity), reads=[bp_], writes=[b_gt])
            chk(6)
            for (dst, bd, q0) in ((r_tm, b_r, 0), (k_tm, b_k, 2), (v_tm, b_v, 4)):
                p_, bp_ = PS()
                for u in range(2):
                    kb.op("pe", lambda e: e.transpose(out=p_[:, u * 128:(u + 1) * 128], in_=st[q0 + u][:, cs], identity=identf[:]),
                          reads=[b_st[q0 + u], b_cst], writes=[bp_])
                kb.op("act", lambda e: e.activation(out=dst[:], in_=p_[:, 0:256], func=AF.Identity), reads=[bp_], writes=[bd])
            kb.op("pool", lambda e: e.tensor_copy(out=v_bf[:], in_=v_tm[:]), reads=[b_v], writes=[b_vb])
            chk(7)
            kb.op("dve", lambda e: e.tensor_tensor(out=kk[:], in0=k_tm[:], in1=vecs[:, 2, :], op=ALU.mult), reads=[b_k, b_cst], writes=[b_kk])
            kb.op("dve", lambda e: e.tensor_tensor(out=t2[:], in0=kk[:], in1=kk[:], op=ALU.mult), reads=[b_kk], writes=[b_t2])
            kb.op("dve", lambda e: e.tensor_reduce(out=ss[:], in_=v3(t2[:]), axis=AX.X, op=ALU.add), reads=[b_t2], writes=[b_ss])
            kb.op("act", lambda e: e.activation(out=ss[:], in_=ss[:], func=AF.Sqrt), reads=[b_ss], writes=[b_ss])
            kb.op("dve", lambda e: e.tensor_scalar(out=ss[:], in0=ss[:], scalar1=1e-12, scalar2=None, op0=ALU.max), reads=[b_ss], writes=[b_ss])
            kb.op("dve", lambda e: e.reciprocal(out=ss[:], in_=ss[:]), reads=[b_ss], writes=[b_ss])
            kb.op("dve", lambda e: e.tensor_tensor(out=v3(kk[:]), in0=v3(kk[:]), in1=bc(ss[:]), op=ALU.mult), reads=[b_kk, b_ss], writes=[b_kk])
            kb.op("dve", lambda e: e.scalar_tensor_tensor(out=t1[:], in0=asig[:], scalar=-1.0, in1=vecs[:, 3, :], op0=ALU.add, op1=ALU.mult),
                  reads=[b_asig, b_cst], writes=[b_t1])
            kb.op("dve", lambda e: e.scalar_tensor_tensor(out=km[:], in0=t1[:], scalar=1.0, in1=k_tm[:], op0=ALU.add, op1=ALU.mult),
                  reads=[b_t1, b_k], writes=[b_km])
            kb.op("dve", lambda e: e.tensor_tensor(out=bv[:], in0=kk[:], in1=asig[:], op=ALU.mult), reads=[b_kk, b_asig], writes=[b_bv])
            kb.op("pool", lambda e: e.tensor_tensor(out=t1[:], in0=r_tm[:], in1=km[:], op=ALU.mult), reads=[b_r, b_km], writes=[b_t1])
            kb.op("pool", lambda e: e.tensor_tensor(out=t1[:], in0=t1[:], in1=vecs[:, 4, :], op=ALU.mult), reads=[b_t1, b_cst], writes=[b_t1])
            kb.op("dve", lambda e: e.tensor_reduce(out=bonus[:], in_=v3(t1[:]), axis=AX.X, op=ALU.add), reads=[b_t1], writes=[b_bonus])
            chk(8)
            pL, bpL = PS()
            kb.op("pe", lambda e: e.matmul(pL[:, 0:256], lhsT=triu[:], rhs=lw[:], start=True, stop=True), reads=[b_lw, b_cst], writes=[bpL])
            kb.op("act", lambda e: e.activation(out=e_pos[:], in_=pL[:, 0:256], func=AF.Exp), reads=[bpL], writes=[b_epos])
            kb.op("act", lambda e: e.activation(out=e_neg[:], in_=pL[:, 0:256], func=AF.Exp, scale=-1.0), reads=[bpL], writes=[b_eneg])
            kb.op("dve", lambda e: e.tensor_tensor(out=t2[:], in0=pL[:, 0:256], in1=lw[:], op=ALU.subtract), reads=[bpL, b_lw], writes=[b_t2])
            kb.op("act", lambda e: e.activation(out=e_prev[:], in_=t2[:], func=AF.Exp), reads=[b_t2], writes=[b_eprev])
            pE, bpE = PS()
            kb.op("pe", lambda e: e.matmul(pE[:, 0:256], lhsT=trils[:], rhs=lw[:], start=True, stop=True), reads=[b_lw, b_cst], writes=[bpE])
            kb.op("act", lambda e: e.activation(out=e_end[:], in_=pE[:, 0:256], func=AF.Exp), reads=[bpE], writes=[b_eend])
            pD, bpD = PS()
            for p in range(2):
                kb.op("pe", lambda e: e.matmul(pD[:, p:p + 1], lhsT=lw[:, p * 128:(p + 1) * 128], rhs=ones[:, 0:1], start=True, stop=True),
                      reads=[b_lw, b_cst], writes=[bpD])
            kb.op("act", lambda e: e.activation(out=dC[:], in_=pD[:, 0:2], func=AF.Exp), reads=[bpD], writes=[b_dC])
            kb.op("dve", lambda e: e.tensor_tensor(out=rt_b[:], in0=r_tm[:], in1=e_pos[:], op=ALU.mult), reads=[b_r, b_epos], writes=[b_rtb])
            kb.op("dve", lambda e: e.scalar_tensor_tensor(out=at_b[:], in0=kk[:], scalar=-1.0, in1=e_prev[:], op0=ALU.mult, op1=ALU.mult),
                  reads=[b_kk, b_eprev], writes=[b_atb])
            kb.op("dve", lambda e: e.tensor_tensor(out=bt_b[:], in0=bv[:], in1=e_neg[:], op=ALU.mult), reads=[b_bv, b_eneg], writes=[b_btb])
            kb.op("pool", lambda e: e.tensor_tensor(out=kt_b[:], in0=km[:], in1=e_neg[:], op=ALU.mult), reads=[b_km, b_eneg], writes=[b_ktb])
            kb.op("dve", lambda e: e.tensor_tensor(out=bh_b[:], in0=bv[:], in1=e_end[:], op=ALU.mult), reads=[b_bv, b_eend], writes=[b_bhb])
            kb.op("pool", lambda e: e.tensor_tensor(out=kh_b[:], in0=km[:], in1=e_end[:], op=ALU.mult), reads=[b_km, b_eend], writes=[b_khb])
            chk(9)
            for p in range(2):
                ps_ = slice(p * 128, (p + 1) * 128)
                ptb, b_ptb = ptbs[p], b_ptbs[p]
                for u, (src, bs) in enumerate(((at_b, b_atb), (rt_b, b_rtb), (kt_b, b_ktb), (bt_b, b_btb))):
                    kb.op("pe", lambda e: e.transpose(out=ptb[:, u, :], in_=src[:, ps_], identity=identb[:]),
                          reads=[bs, b_cst], writes=[b_ptb])
                chk(9.3 + p)
                kb.op("dve", lambda e: e.tensor_copy(out=AR[p][:, 0, :], in_=ptb[:, 0, :]), reads=[b_ptb], writes=[b_AR[p]])
                chk(9.5 + p)
                kb.op("dve", lambda e: e.tensor_copy(out=AR[p][:, 1, :], in_=ptb[:, 1, :]), reads=[b_ptb], writes=[b_AR[p]])
                kb.op("dve", lambda e: e.tensor_copy(out=KT[p][:], in_=ptb[:, 2, :]), reads=[b_ptb], writes=[b_KT[p]])
                kb.op("dve", lambda e: e.tensor_copy(out=BT[p][:], in_=ptb[:, 3, :]), reads=[b_ptb], writes=[b_BT[p]])
            chk(10)
            for h in range(4):
                p, hh = h // 2, h % 2
                sl = slice(64 * hh, 64 * hh + 64)
                hc = slice(64 * h, 64 * h + 64)
                p1, bp1 = PS()
                kb.op("pe", lambda e: e.matmul(p1[:, 0:256], lhsT=KT[p][sl, :], rhs=AR[p][sl, :, :], start=True, stop=True),
                      reads=[b_KT[p], b_AR[p]], writes=[bp1])
                kb.op("dve", lambda e: e.tensor_tensor(out=HG2[:], in0=p1[:, 0:256], in1=msk[:], op=ALU.mult), reads=[bp1, b_cst], writes=[b_HG2])
                p2, bp2 = PS()
                kb.op("pe", lambda e: e.matmul(p2[:, 0:256], lhsT=BT[p][sl, :], rhs=AR[p][sl, :, :], start=True, stop=True),
                      reads=[b_BT[p], b_AR[p]], writes=[bp2])
                kb.op("dve", lambda e: e.tensor_tensor(out=Nc[0][:], in0=p2[:, 0:128], in1=msk[:, 0:128], op=ALU.mult), reads=[bp2, b_cst], writes=[b_Nc[0]])
                kb.op("dve", lambda e: e.tensor_tensor(out=G1m[:], in0=p2[:, 128:256], in1=msk[:, 128:256], op=ALU.mult), reads=[bp2, b_cst], writes=[b_G1m])
                p3, bp3 = PS()
                kb.op("pe", lambda e: e.matmul(p3[:, 0:128], lhsT=AR[p][sl, 0, :], rhs=BT[p][sl, :], start=True, stop=True),
                      reads=[b_BT[p], b_AR[p]], writes=[bp3])
                kb.op("dve", lambda e: e.tensor_tensor(out=Lc[0][:], in0=p3[:, 0:128], in1=mls[:], op=ALU.mult), reads=[bp3, b_cst], writes=[b_Lc[0]])
                kb.op("pool", lambda e: e.tensor_tensor(out=Pm[0][:], in0=Nc[0][:], in1=identf[:], op=ALU.add), reads=[b_Nc[0], b_cst], writes=[b_Pm[0]])
                cur = 0
                for it in range(6):
                    nxt = 1 - cur
                    pl, bpl = PS()
                    kb.op("pe", lambda e: e.matmul(pl[:, 0:128], lhsT=Nc[cur][:], rhs=Lc[cur][:], start=True, stop=True),
                          reads=[b_Nc[cur], b_Lc[cur]], writes=[bpl])
                    if it < 5:
                        pn, bpn = PS()
                        kb.op("pe", lambda e: e.matmul(pn[:, 0:128], lhsT=Lc[cur][:], rhs=Nc[cur][:], start=True, stop=True),
                              reads=[b_Nc[cur], b_Lc[cur]], writes=[bpn])
                    kb.op("act", lambda e: e.activation(out=Lc[nxt][:], in_=pl[:, 0:128], func=AF.Identity), reads=[bpl], writes=[b_Lc[nxt]])
                    if it < 5:
                        kb.op("dve", lambda e: e.tensor_copy(out=Nc[nxt][:], in_=pn[:, 0:128]), reads=[bpn], writes=[b_Nc[nxt]])
                    pq, bpq = PS()
                    kb.op("pe", lambda e: e.matmul(pq[:, 0:128], lhsT=Lc[nxt][:], rhs=Pm[cur][:], start=True, stop=True),
                          reads=[b_Lc[nxt], b_Pm[cur]], writes=[bpq])
                    kb.op("dve", lambda e: e.tensor_tensor(out=Pm[nxt][:], in0=pq[:, 0:128], in1=Pm[cur][:], op=ALU.add),
                          reads=[bpq, b_Pm[cur]], writes=[b_Pm[nxt]])
                    cur = nxt
                pw, bpw = PS()
                kb.op("pe", lambda e: e.matmul(pw[:, 0:64], lhsT=AR[p][sl, 0, :], rhs=Sb[p][sl, :], start=True, stop=False),
                      reads=[b_AR[p], b_S[p]], writes=[bpw])
                kb.op("pe", lambda e: e.matmul(pw[:, 0:64], lhsT=HG2[:, 0:128], rhs=v_bf[:, hc], start=False, stop=True),
                      reads=[b_HG2, b_vb], writes=[bpw])
                kb.op("act", lambda e: e.activation(out=W0[:], in_=pw[:, 0:64], func=AF.Identity), reads=[bpw], writes=[b_W0])
                pu, bpu = PS()
                kb.op("pe", lambda e: e.matmul(pu[:, 0:64], lhsT=Pm[cur][:], rhs=W0[:], start=True, stop=True),
                      reads=[b_Pm[cur], b_W0], writes=[bpu])
                kb.op("act", lambda e: e.activation(out=Ub[:, hc], in_=pu[:, 0:64], func=AF.Identity), reads=[bpu], writes=[b_Ub])
                py, bpy = PS()
                kb.op("pe", lambda e: e.matmul(py[:, 0:64], lhsT=AR[p][sl, 1, :], rhs=Sb[p][sl, :], start=True, stop=False),
                      reads=[b_AR[p], b_S[p]], writes=[bpy])
                kb.op("pe", lambda e: e.matmul(py[:, 0:64], lhsT=G1m[:], rhs=Ub[:, hc], start=False, stop=False),
                      reads=[b_G1m, b_Ub], writes=[bpy])
                kb.op("pe", lambda e: e.matmul(py[:, 0:64], lhsT=HG2[:, 128:256], rhs=v_bf[:, hc], start=False, stop=True),
                      reads=[b_HG2, b_vb], writes=[bpy])
                kb.op("act", lambda e: e.activation(out=y_tm[:, hc], in_=py[:, 0:64], func=AF.Identity), reads=[bpy], writes=[b_y])
            chk(11)
            for p in range(2):
                pc = slice(p * 128, (p + 1) * 128)
                pS_, bpS = PS()
                kb.op("pe", lambda e: e.matmul(pS_[:, 0:128], lhsT=bh_b[:, pc], rhs=Ub[:, pc], start=True, stop=False),
                      reads=[b_bhb, b_Ub], writes=[bpS])
                kb.op("pe", lambda e: e.matmul(pS_[:, 0:128], lhsT=kh_b[:, pc], rhs=v_bf[:, pc], start=False, stop=True),
                      reads=[b_khb, b_vb], writes=[bpS])
                for hh in range(2):
                    sl = slice(64 * hh, 64 * hh + 64)
                    kb.op("dve", lambda e: e.scalar_tensor_tensor(out=St[p][sl, :], in0=St[p][sl, :], scalar=dC[sl, p:p + 1],
                                                                  in1=pS_[sl, 64 * hh:64 * hh + 64], op0=ALU.mult, op1=ALU.add),
                          reads=[b_S[p], b_dC, bpS], writes=[b_S[p]])
                kb.op("pool", lambda e: e.tensor_copy(out=Sb[p][:], in_=St[p][:]), reads=[b_S[p]], writes=[b_S[p]])
            chk(12)
            kb.op("dve", lambda e: e.tensor_reduce(out=s1[:], in_=v3(y_tm[:]), axis=AX.X, op=ALU.add), reads=[b_y], writes=[b_s1])
            kb.op("dve", lambda e: e.tensor_scalar(out=s1[:], in0=s1[:], scalar1=-1.0 / 64, scalar2=None, op0=ALU.mult), reads=[b_s1], writes=[b_s1])
            kb.op("dve", lambda e: e.tensor_tensor(out=v3(yc[:]), in0=v3(y_tm[:]), in1=bc(s1[:]), op=ALU.add), reads=[b_y, b_s1], writes=[b_yc])
            kb.op("pool", lambda e: e.tensor_tensor(out=t2[:], in0=yc[:], in1=yc[:], op=ALU.mult), reads=[b_yc], writes=[b_t2])
            kb.op("dve", lambda e: e.tensor_reduce(out=s2[:], in_=v3(t2[:]), axis=AX.X, op=ALU.add), reads=[b_t2], writes=[b_s2])
            kb.op("act", lambda e: e.activation(out=s2[:], in_=s2[:], func=AF.Sqrt, scale=1.0 / 64, bias=64e-5), reads=[b_s2], writes=[b_s2])
            kb.op("dve", lambda e: e.reciprocal(out=s2[:], in_=s2[:]), reads=[b_s2], writes=[b_s2])
            kb.op("dve", lambda e: e.tensor_tensor(out=v3(yc[:]), in0=v3(yc[:]), in1=bc(s2[:]), op=ALU.mult), reads=[b_yc, b_s2], writes=[b_yc])
            kb.op("pool", lambda e: e.tensor_tensor(out=yc[:], in0=yc[:], in1=vecs[:, 5, :], op=ALU.mult), reads=[b_yc, b_cst], writes=[b_yc])
            kb.op("pool", lambda e: e.tensor_tensor(out=yc[:], in0=yc[:], in1=vecs[:, 6, :], op=ALU.add), reads=[b_yc, b_cst], writes=[b_yc])
            kb.op("dve", lambda e: e.tensor_tensor(out=v3(t1[:]), in0=v3(v_tm[:]), in1=bc(bonus[:]), op=ALU.mult), reads=[b_v, b_bonus], writes=[b_t1])
            kb.op("dve", lambda e: e.tensor_tensor(out=yc[:], in0=yc[:], in1=t1[:], op=ALU.add), reads=[b_yc, b_t1], writes=[b_yc])
            kb.op("dve", lambda e: e.tensor_tensor(out=oa[:], in0=yc[:], in1=gt[:], op=ALU.mult), reads=[b_yc, b_gt], writes=[b_oa])
            if not fm_out:
                ob = Buf("o"); outs.append(ob)
                kb.dma("sp", o_a[t0 + c0:t0 + c0 + 128, :], oa[:], reads=[b_oa], writes=[ob])
            else:
                p_, bp_ = PS()
                for u in range(2):
                    kb.op("pe", lambda e: e.transpose(out=p_[:, u * 128:(u + 1) * 128], in_=oa[:, u * 128:(u + 1) * 128], identity=identf[:]),
                          reads=[b_oa, b_cst], writes=[bp_])
                kb.op("act", lambda e: e.activation(out=yc[:], in_=p_[:, 0:256], func=AF.Identity), reads=[bp_], writes=[b_yc])
                for u in range(2):
                    ob = Buf("o"); outs.append(ob)
                    kb.dma("sp", o_a[u * 128:(u + 1) * 128, t0 + c0:t0 + c0 + 128], yc[:, u * 128:(u + 1) * 128], reads=[b_yc], writes=[ob])


def attn_const_inputs(nc, S):
    d = {}
    for nm, shp in (("cos", [128, S]), ("sin", [128, S]), ("bones", [128, 128]), ("perm", [128, 128]), ("ident", [128, 128]),
                    ("dmask", [128, 4, 512]), ("gq", [128, 1]), ("gk", [128, 1]), ("gs", [128, 1]), ("lam", [1, 256])):
        d[nm] = nc.dram_tensor("a_" + nm, shp, F32, kind="ExternalInput").ap()
    return d


def attn_const_host(S):
    half = 8
    inv_freq = (500000.0 ** (-np.arange(half, dtype=np.float32) * 2.0 / 16)).astype(np.float32)
    ang = np.arange(S, dtype=np.float32)[None, :] * inv_freq[:, None]
    cos = np.ones((64, S), np.float32); sin = np.zeros((64, S), np.float32)
    cos[0:8] = np.cos(ang); cos[8:16] = np.cos(ang); sin[0:8] = np.sin(ang); sin[8:16] = np.sin(ang)
    bones = np.zeros((128, 128), np.float32); bones[:64, :64] = 1; bones[64:, 64:] = 1
    perm = np.zeros((128, 128), np.float32)
    for g in (0, 64):
        for i in range(8):
            perm[g + i + 8, g + i] = -1.0
            perm[g + i, g + i + 8] = 1.0
    j = np.arange(128)[:, None]; i = np.arange(512)[None, :]
    dmask = np.stack([((d * 128 + j) // 64 <= i // 64) for d in range(4)], axis=1).astype(np.float32)
    return {"a_cos": np.tile(cos, (2, 1)), "a_sin": np.tile(sin, (2, 1)), "a_bones": bones, "a_perm": perm,
            "a_ident": np.eye(128, dtype=np.float32), "a_dmask": dmask}


def phase_attn(nc, kb, Scope, projT, S, cst, o_bT, outs, row0=1216):
    QK = nc.dram_tensor("att_qk", [8, 65, S], BF16).ap()
    Vd = nc.dram_tensor("att_v", [S, 256], BF16).ap()
    TB = min(512, S)
    NB = S // TB
    b_QK = Buf("att_qk"); b_Vd = Buf("att_v")
    A = Scope()
    b_cst = Buf("acst")
    bones = A.sb("bones", [128, 128], F32); perm = A.sb("perm", [128, 128], F32); identf = A.sb("a_identf", [128, 128], F32)
    gq = A.sb("gq", [128, 1], F32); gk = A.sb("gk", [128, 1], F32)
    for tile, nm in ((bones, "bones"), (perm, "perm"), (identf, "ident"), (gq, "gq"), (gk, "gk")):
        kb.dma("sp", tile[:], cst[nm], writes=[b_cst])
    kb.op("dve", lambda e: e.tensor_scalar(out=gq[:], in0=gq[:], scalar1=0.125, scalar2=None, op0=ALU.mult), reads=[b_cst], writes=[b_cst])
    onesr = A.sb("onesr", [1, TB], BF16)
    kb.op("dve", lambda e: e.memset(onesr[:], 1.0), writes=[b_cst])
    kmaxb = [A.sb(f"kmaxb{u}", [128, TB], F32) for u in range(2)]; b_kmaxb = [Buf("kmaxb0"), Buf("kmaxb1")]
    kmax = A.sb("kmax", [128, 2], F32); b_kmax = Buf("kmax")
    for u in range(2):
        kb.op("pool", lambda e: e.memset(kmaxb[u][:], 0.0), writes=[b_kmaxb[u]])
    xq = [A.sb(f"xq{i}", [128, TB], F32) for i in range(2)]; b_xq = [Buf("xq0"), Buf("xq1")]
    cs_t = A.sb("cs_t", [128, 2, TB], F32); b_cs = Buf("cs")
    sq, b_sq = A.sb("a_sq", [128, TB], F32), Buf("a_sq")
    rstd, b_rstd = A.sb("a_rstd", [128, TB], F32), Buf("a_rstd")
    xn, b_xn = A.sb("a_xn", [128, TB], F32), Buf("a_xn")
    ta, b_ta = A.sb("a_ta", [128, TB], F32), Buf("a_ta")
    rot, b_rot = A.sb("a_rot", [128, TB], F32), Buf("a_rot")
    rotb, b_rotb = A.sb("a_rotb", [128, TB], BF16), Buf("a_rotb")
    shf, b_shf = A.sb("a_shf", [128, TB], BF16), Buf("a_shf")
    vtb, b_vtb = A.sb("a_vtb", [128, 256], BF16), Buf("a_vtb")
    pp = [A.ps(f"aps{i}", [128, 512], F32) for i in range(4)]; b_pp = [Buf(f"aps{i}") for i in range(4)]
    pc = [0]

    def PS():
        i = pc[0] % 4
        pc[0] += 1
        return pp[i], b_pp[i]

    cnt = 0
    for which in ("k", "q"):
        base = row0 + (256 if which == "k" else 0)
        gain = gk if which == "k" else gq
        for tb in range(NB):
            ts_ = slice(tb * TB, (tb + 1) * TB)
            kb.dma("act", cs_t[:, 0, :], cst["cos"][:, ts_], writes=[b_cs])
            kb.dma("act", cs_t[:, 1, :], cst["sin"][:, ts_], writes=[b_cs])
            for u in range(2):
                i = cnt % 2
                cnt += 1
                kb.dma("sp", xq[i][:], projT[base + u * 128:base + (u + 1) * 128, ts_], writes=[b_xq[i]])
                kb.op("act", lambda e: e.activation(out=sq[:], in_=xq[i][:], func=AF.Square), reads=[b_xq[i]], writes=[b_sq])
                p_, bp_ = PS()
                kb.op("pe", lambda e: e.matmul(p_[:, :TB], lhsT=bones[:], rhs=sq[:], start=True, stop=True), reads=[b_sq, b_cst], writes=[bp_])
                kb.op("act", lambda e: e.activation(out=rstd[:], in_=p_[:, :TB], func=AF.Sqrt, scale=1.0 / 64, bias=1e-6), reads=[bp_], writes=[b_rstd])
                kb.op("dve", lambda e: e.reciprocal(out=rstd[:], in_=rstd[:]), reads=[b_rstd], writes=[b_rstd])
                kb.op("dve", lambda e: e.scalar_tensor_tensor(out=xn[:], in0=xq[i][:], scalar=gain[:, 0:1], in1=rstd[:], op0=ALU.mult, op1=ALU.mult),
                      reads=[b_xq[i], b_rstd, b_cst], writes=[b_xn])
                p2, bp2 = PS()
                kb.op("pe", lambda e: e.matmul(p2[:, :TB], lhsT=perm[:], rhs=xn[:], start=True, stop=True), reads=[b_xn, b_cst], writes=[bp2])
                kb.op("pool", lambda e: e.tensor_tensor(out=ta[:], in0=xn[:], in1=cs_t[:, 0, :], op=ALU.mult), reads=[b_xn, b_cs], writes=[b_ta])
                kb.op("dve", lambda e: e.tensor_tensor(out=rot[:], in0=p2[:, :TB], in1=cs_t[:, 1, :], op=ALU.mult), reads=[bp2, b_cs], writes=[b_rot])
                kb.op("dve", lambda e: e.tensor_tensor(out=rot[:], in0=rot[:], in1=ta[:], op=ALU.add), reads=[b_rot, b_ta], writes=[b_rot])
                kb.op("pool", lambda e: e.tensor_copy(out=rotb[:], in_=rot[:]), reads=[b_rot], writes=[b_rotb])
                kb.op("act", lambda e: e.activation(out=sq[:], in_=rot[:], func=AF.Square), reads=[b_rot], writes=[b_sq])
                p3, bp3 = PS()
                kb.op("pe", lambda e: e.matmul(p3[:, :TB], lhsT=bones[:], rhs=sq[:], start=True, stop=True), reads=[b_sq, b_cst], writes=[bp3])
                qi = (0 if which == "q" else 4) + u * 2
                for m in range(2):
                    ob = Buf("o")
                    kb.dma("sp", QK[qi + m, 0:64, ts_], rotb[64 * m:64 * m + 64, :], reads=[b_rotb], writes=[ob, b_QK])
                if which == "k":
                    kb.op("dve", lambda e: e.tensor_tensor(out=kmaxb[u][:], in0=kmaxb[u][:], in1=p3[:, :TB], op=ALU.max), reads=[bp3, b_kmaxb[u]], writes=[b_kmaxb[u]])
                    for m in range(2):
                        kb.dma("sp", QK[qi + m, 64:65, ts_], onesr[0:1, :], reads=[b_cst], writes=[b_QK])
                else:
                    kb.op("dve", lambda e: e.tensor_scalar(out=ta[:], in0=p3[:, :TB], scalar1=kmax[:, u:u + 1], scalar2=None, op0=ALU.mult),
                          reads=[bp3, b_kmax], writes=[b_ta])
                    kb.op("act", lambda e: e.activation(out=ta[:], in_=ta[:], func=AF.Sqrt), reads=[b_ta], writes=[b_ta])
                    kb.op("dve", lambda e: e.tensor_scalar(out=shf[:], in0=ta[:], scalar1=-1.0, scalar2=None, op0=ALU.mult), reads=[b_ta], writes=[b_shf])
                    for m in range(2):
                        kb.dma("sp", QK[qi + m, 64:65, ts_], shf[64 * m:64 * m + 1, :], reads=[b_shf], writes=[b_QK])
        if which == "k":
            for u in range(2):
                kb.op("dve", lambda e: e.tensor_reduce(out=kmax[:, u:u + 1], in_=kmaxb[u][:], axis=AX.X, op=ALU.max), reads=[b_kmaxb[u]], writes=[b_kmax])
    for tb in range(NB):
        ts_ = slice(tb * TB, (tb + 1) * TB)
        for u in range(2):
            i = cnt % 2
            cnt += 1
            kb.dma("sp", xq[i][:], projT[row0 + 512 + u * 128:row0 + 512 + (u + 1) * 128, ts_], writes=[b_xq[i]])
            for c4 in range(TB // 128):
                p_, bp_ = PS()
                kb.op("pe", lambda e: e.transpose(out=p_[:, 0:128], in_=xq[i][:, c4 * 128:(c4 + 1) * 128], identity=identf[:]), reads=[b_xq[i], b_cst], writes=[bp_])
                kb.op("dve", lambda e: e.tensor_copy(out=vtb[:, 0:128], in_=p_[:, 0:128]), reads=[bp_], writes=[b_vtb])
                t0 = tb * TB + c4 * 128
                kb.dma("sp", Vd[t0:t0 + 128, u * 128:(u + 1) * 128], vtb[:, 0:128], reads=[b_vtb], writes=[b_Vd])
    A.close()
    M = Scope()
    b_c2 = Buf("acst2")
    dmf = M.sb("dmf", [128, 4, 512], F32); dm = M.sb("dm", [128, 4, 512], BF16)
    kb.dma("sp", dmf[:], cst["dmask"], writes=[b_c2])
    kb.op("dve", lambda e: e.tensor_copy(out=dm[:], in_=dmf[:]), reads=[b_c2], writes=[b_c2])
    onesb = M.sb("onesb", [128, 128], BF16); onesf = M.sb("onesf", [128, 128], F32)
    kb.op("dve", lambda e: e.memset(onesb[:], 1.0), writes=[b_c2])
    kb.op("dve", lambda e: e.memset(onesf[:], 1.0), writes=[b_c2])
    gs = M.sb("gs", [128, 1], F32)
    kb.dma("sp", gs[:], cst["gs"], writes=[b_c2])
    kb.op("dve", lambda e: e.tensor_scalar(out=gs[:], in0=gs[:], scalar1=0.8, scalar2=None, op0=ALU.mult), reads=[b_c2], writes=[b_c2])
    lamr = M.sb("lamr", [1, 256], F32); lam2 = M.sb("lam2", [1, 2], F32); lam1 = M.sb("lam1", [1, 1], F32); nlam = M.sb("nlam", [128, 1], F32)
    kb.dma("sp", lamr[:], cst["lam"], writes=[b_c2])
    kb.op("dve", lambda e: e.tensor_tensor(out=lamr[:, 0:64], in0=lamr[:, 0:64], in1=lamr[:, 64:128], op=ALU.mult), reads=[b_c2], writes=[b_c2])
    kb.op("dve", lambda e: e.tensor_tensor(out=lamr[:, 128:192], in0=lamr[:, 128:192], in1=lamr[:, 192:256], op=ALU.mult), reads=[b_c2], writes=[b_c2])
    kb.op("dve", lambda e: e.tensor_reduce(out=lam2[:], in_=lamr[:].rearrange("p (a b) -> p a b", a=2)[:, :, 0:64], axis=AX.X, op=ALU.add), reads=[b_c2], writes=[b_c2])
    kb.op("act", lambda e: e.activation(out=lam2[:], in_=lam2[:], func=AF.Exp), reads=[b_c2], writes=[b_c2])
    kb.op("dve", lambda e: e.tensor_tensor(out=lam1[:], in0=lam2[:, 1:2], in1=lam2[:, 0:1], op=ALU.subtract), reads=[b_c2], writes=[b_c2])
    kb.op("dve", lambda e: e.tensor_scalar(out=lam1[:], in0=lam1[:], scalar1=-0.2, scalar2=None, op0=ALU.add), reads=[b_c2], writes=[b_c2])
    pl = M.ps("apl", [128, 512], F32); b_pl = Buf("apl")
    kb.op("pe", lambda e: e.matmul(pl[:, 0:1], lhsT=onesf[0:1, :], rhs=lam1[0:1, 0:1], start=True, stop=True), reads=[b_c2], writes=[b_pl])
    kb.op("dve", lambda e: e.tensor_copy(out=nlam[:], in_=pl[:, 0:1]), reads=[b_pl], writes=[b_c2])

    Kr = M.sb("Kr", [128, 2, S], BF16); b_Kr = Buf("Kr")
    Vr = M.sb("Vr", [128, S // 128, 128], BF16); b_Vr = Buf("Vr")
    Qb = [M.sb(f"Qb{i}", [128, 2, TB], BF16) for i in range(2)]; b_Qb = [Buf("Qb0"), Buf("Qb1")]
    PT = [M.sb(f"PT{i}", [128, TB], BF16) for i in range(4)]; b_PT = [Buf(f"PT{i}") for i in range(4)]
    ps_s = [M.ps(f"ps_s{i}", [128, 512], F32) for i in range(2)]; b_ps_s = [Buf("pss0"), Buf("pss1")]
    ps_o = [M.ps(f"ps_o{i}", [128, 512], F32) for i in range(2)]; b_ps_o = [Buf("pso0"), Buf("pso1")]
    ps_l = [M.ps(f"ps_l{i}", [128, 512], F32) for i in range(2)]; b_ps_l = [Buf("psl0"), Buf("psl1")]
    o1, b_o1 = M.sb("o1", [128, TB], F32), Buf("o1")
    o2, b_o2 = M.sb("o2", [128, TB], F32), Buf("o2")
    rl, b_rl = M.sb("rl", [128, TB], F32), Buf("rl")
    osq, b_osq = M.sb("osq", [128, TB], F32), Buf("osq")
    ofin = [M.sb(f"ofin{i}", [128, TB], F32) for i in range(2)]; b_ofin = [Buf("ofin0"), Buf("ofin1")]
    sct = 0
    pct = 0
    for u in range(2):
        for m in range(2):
            kb.dma("sp", Kr[0:65, m, :], QK[4 + u * 2 + m, :, :], reads=[b_QK], writes=[b_Kr])
        kb.dma("act", Vr[:], Vd[:, u * 128:(u + 1) * 128].rearrange("(n p) d -> p n d", p=128), reads=[b_Vd], writes=[b_Vr])
        for qb in range(NB):
            qi = (u * NB + qb) % 2
            q0 = qb * TB
            for m in range(2):
                kb.dma("sp", Qb[qi][0:65, m, :], QK[u * 2 + m, :, q0:q0 + TB], reads=[b_QK], writes=[b_Qb[qi]])
            nkb = (q0 + TB) // 128
            for kbk in range(nkb):
                k0 = kbk * 128
                diag = k0 >= q0
                for m in range(2):
                    si = sct % 2; sct += 1
                    pi = pct % 4; pct += 1
                    kb.op("pe", lambda e: e.matmul(ps_s[si][:, :TB], lhsT=Kr[0:65, m, k0:k0 + 128], rhs=Qb[qi][0:65, m, :], start=True, stop=True),
                          reads=[b_Kr, b_Qb[qi]], writes=[b_ps_s[si]])
                    kb.op("act", lambda e: e.activation(out=PT[pi][:], in_=ps_s[si][:, :TB], func=AF.Exp), reads=[b_ps_s[si]], writes=[b_PT[pi]])
                    if diag:
                        dd = (k0 - q0) // 128
                        kb.op("pool", lambda e: e.tensor_tensor(out=PT[pi][:], in0=PT[pi][:], in1=dm[:, dd, :TB], op=ALU.mult), reads=[b_PT[pi], b_c2], writes=[b_PT[pi]])
                    kb.op("pe", lambda e: e.matmul(ps_o[m][:, :TB], lhsT=Vr[:, kbk, :], rhs=PT[pi][:], start=(kbk == 0), stop=(kbk == nkb - 1)),
                          reads=[b_Vr, b_PT[pi]], writes=[b_ps_o[m]])
                    kb.op("pe", lambda e: e.matmul(ps_l[m][:, :TB], lhsT=onesb[:], rhs=PT[pi][:], start=(kbk == 0), stop=(kbk == nkb - 1)),
                          reads=[b_c2, b_PT[pi]], writes=[b_ps_l[m]])
            kb.op("dve", lambda e: e.reciprocal(out=rl[:], in_=ps_l[0][:, :TB]), reads=[b_ps_l[0]], writes=[b_rl])
            kb.op("dve", lambda e: e.tensor_tensor(out=o1[:], in0=ps_o[0][:, :TB], in1=rl[:], op=ALU.mult), reads=[b_ps_o[0], b_rl], writes=[b_o1])
            kb.op("dve", lambda e: e.reciprocal(out=rl[:], in_=ps_l[1][:, :TB]), reads=[b_ps_l[1]], writes=[b_rl])
            kb.op("dve", lambda e: e.tensor_tensor(out=o2[:], in0=ps_o[1][:, :TB], in1=rl[:], op=ALU.mult), reads=[b_ps_o[1], b_rl], writes=[b_o2])
            kb.op("dve", lambda e: e.scalar_tensor_tensor(out=o1[:], in0=o2[:], scalar=nlam[:, 0:1], in1=o1[:], op0=ALU.mult, op1=ALU.add),
                  reads=[b_o1, b_o2, b_c2], writes=[b_o1])
            kb.op("act", lambda e: e.activation(out=osq[:], in_=o1[:], func=AF.Square), reads=[b_o1], writes=[b_osq])
            kb.op("pe", lambda e: e.matmul(pl[:, :TB], lhsT=onesf[:], rhs=osq[:], start=True, stop=True), reads=[b_osq, b_c2], writes=[b_pl])
            kb.op("act", lambda e: e.activation(out=rl[:], in_=pl[:, :TB], func=AF.Sqrt, scale=1.0 / 128, bias=1e-6), reads=[b_pl], writes=[b_rl])
            kb.op("dve", lambda e: e.reciprocal(out=rl[:], in_=rl[:]), reads=[b_rl], writes=[b_rl])
            oi = (u * NB + qb) % 2
            kb.op("dve", lambda e: e.scalar_tensor_tensor(out=ofin[oi][:], in0=o1[:], scalar=gs[:, 0:1], in1=rl[:], op0=ALU.mult, op1=ALU.mult),
                  reads=[b_o1, b_rl, b_c2], writes=[b_ofin[oi]])
            ob = Buf("o"); outs.append(ob)
            kb.dma("sp", o_bT[u * 128:(u + 1) * 128, q0:q0 + TB], ofin[oi][:], reads=[b_ofin[oi]], writes=[ob])
    M.close()


def build_attn_test(S):
    nc = bass.Bass("TRN2", target_bir_lowering=False)
    projT = nc.dram_tensor("projT", [768, S], F32, kind="ExternalInput").ap()
    o_bT = nc.dram_tensor("o_bT", [256, S], F32, kind="ExternalOutput").ap()
    cst = attn_const_inputs(nc, S)
    kb = KB(nc)
    Scope = make_scope(nc, kb)
    outs = []
    phase_attn(nc, kb, Scope, projT, S, cst, o_bT, outs, row0=0)
    kb.drain("sp", outs)
    return nc


def gemm(nc, kb, Scope, W, K, N, XT, T, mode, epi, x_f32=False, NG=512, tag="g", xload=None):
    G = Scope()
    KC = K // 128
    TB = min(512, T)
    Wv = W.rearrange("(kc p) n -> p kc n", p=128)
    Xv = XT.rearrange("(kc p) t -> p kc t", p=128) if XT is not None else None
    Wf = G.sb(tag + "Wf", [128, KC, NG], F32); b_Wf = Buf("Wf")
    Wb = [G.sb(tag + f"Wb{i}", [128, KC, NG], BF16) for i in range(2)]; b_Wb = [Buf("Wb0"), Buf("Wb1")]
    Xb = [G.sb(tag + f"Xb{i}", [128, KC, TB], BF16) for i in range(2)]; b_Xb = [Buf("Xb0"), Buf("Xb1")]
    if x_f32:
        Xf = G.sb(tag + "Xf", [128, KC, TB], F32); b_Xf = Buf("Xf")
    pp = [G.ps(tag + f"ps{i}", [128, 512], F32) for i in range(4)]; b_pp = [Buf(f"gps{i}") for i in range(4)]
    pc = 0
    xc = 0
    for gi, n0 in enumerate(range(0, N, NG)):
        ng = min(NG, N - n0)
        wi = gi % 2
        kb.dma("act", Wf[:, :, :ng], Wv[:, :, n0:n0 + ng], writes=[b_Wf])
        h = KC // 2
        kb.op("pool", lambda e: e.tensor_copy(out=Wb[wi][:, :h, :ng], in_=Wf[:, :h, :ng]), reads=[b_Wf], writes=[b_Wb[wi]])
        kb.op("dve", lambda e: e.tensor_copy(out=Wb[wi][:, h:, :ng], in_=Wf[:, h:, :ng]), reads=[b_Wf], writes=[b_Wb[wi]])
        for t0 in range(0, T, TB):
            xi = xc % 2
            xc += 1
            if xload is not None:
                xload(t0, Xf, b_Xf)
                kb.op("pool", lambda e: e.tensor_copy(out=Xb[xi][:], in_=Xf[:]), reads=[b_Xf], writes=[b_Xb[xi]])
            elif x_f32:
                kb.dma("sp", Xf[:], Xv[:, :, t0:t0 + TB], writes=[b_Xf])
                kb.op("pool", lambda e: e.tensor_copy(out=Xb[xi][:], in_=Xf[:]), reads=[b_Xf], writes=[b_Xb[xi]])
            else:
                kb.dma("sp", Xb[xi][:], Xv[:, :, t0:t0 + TB], writes=[b_Xb[xi]])
            if mode == "fm":
                for nb in range(0, ng, 128):
                    nw = min(128, ng - nb)
                    pi = pc % 4; pc += 1
                    for k in range(KC):
                        kb.op("pe", lambda e: e.matmul(pp[pi][:nw, :TB], lhsT=Wb[wi][:, k, nb:nb + nw], rhs=Xb[xi][:, k, :],
                                                       start=(k == 0), stop=(k == KC - 1)),
                              reads=[b_Wb[wi], b_Xb[xi]], writes=[b_pp[pi]])
                    epi(n0 + nb, nw, t0, TB, pp[pi], b_pp[pi])
            else:
                for ts in range(0, TB, 128):
                    pi = pc % 4; pc += 1
                    for k in range(KC):
                        kb.op("pe", lambda e: e.matmul(pp[pi][:, :ng], lhsT=Xb[xi][:, k, ts:ts + 128], rhs=Wb[wi][:, k, :ng],
                                                       start=(k == 0), stop=(k == KC - 1)),
                              reads=[b_Wb[wi], b_Xb[xi]], writes=[b_pp[pi]])
                    epi(t0 + ts, 128, n0, ng, pp[pi], b_pp[pi])
    G.close()


def norm_to_fm(nc, kb, Scope, xsrc, T, hs, b_hs, hb_ap, b_hb, ident, b_ident, hT_out, b_out, tag="n"):
    Pn = Scope()
    KC = D // 128
    xt = [Pn.sb(tag + f"xt{i}", [128, D], F32) for i in range(2)]; b_xt = [Buf("xt0"), Buf("xt1")]
    xs = [Pn.sb(tag + f"xs{i}", [128, D], BF16) for i in range(2)]; b_xs = [Buf("xs0"), Buf("xs1")]
    junk = Pn.sb(tag + "junk", [128, D], BF16); b_junk = Buf("junk")
    ssq = [Pn.sb(tag + f"ssq{i}", [128, 1], F32) for i in range(2)]; b_ssq = [Buf("ssq0"), Buf("ssq1")]
    ptr = [Pn.ps(tag + f"ptr{i}", [128, 4, 128], BF16) for i in range(2)]; b_ptr = [Buf("ptr0"), Buf("ptr1")]
    ht = [Pn.sb(tag + f"ht{i}", [128, KC, 128], BF16) for i in range(2)]; b_ht = [Buf("ht0"), Buf("ht1")]
    hv = hT_out.rearrange("(kc p) t -> p kc t", p=128)
    for t in range(T // 128):
        i = t % 2
        kb.dma("sp", xt[i][:], xsrc[t * 128:(t + 1) * 128, :], writes=[b_xt[i]])
        kb.op("act", lambda e: e.activation(out=junk[:], in_=xt[i][:], func=AF.Square, accum_out=ssq[i][:]),
              reads=[b_xt[i]], writes=[b_junk, b_ssq[i]])
        kb.op("act", lambda e: e.activation(out=ssq[i][:], in_=ssq[i][:], func=AF.Sqrt, scale=1.0 / D, bias=1e-6),
              reads=[b_ssq[i]], writes=[b_ssq[i]])
        kb.op("dve", lambda e: e.reciprocal(out=ssq[i][:], in_=ssq[i][:]), reads=[b_ssq[i]], writes=[b_ssq[i]])
        kb.op("dve", lambda e: e.tensor_scalar(out=xs[i][:], in0=xt[i][:], scalar1=ssq[i][:, 0:1], scalar2=None,
                                               op0=ALU.mult), reads=[b_xt[i], b_ssq[i]], writes=[b_xs[i]])
        for q in range(KC // 4):
            pi = q % 2
            for u in range(4):
                k = q * 4 + u
                kb.op("pe", lambda e: e.transpose(out=ptr[pi][:, u, :], in_=xs[i][:, k * 128:(k + 1) * 128], identity=ident[:]),
                      reads=[b_xs[i], b_ident], writes=[b_ptr[pi]])
            for u in range(4):
                k = q * 4 + u
                kb.op("act", lambda e: e.activation(out=ht[i][:, k, :], in_=ptr[pi][:, u, :], func=AF.Identity,
                                                    scale=hs[:, k:k + 1], bias=hb_ap[:, k:k + 1]),
                      reads=[b_ptr[pi], b_hs, b_hb], writes=[b_ht[i]])
        kb.dma("sp", hv[:, :, t * 128:(t + 1) * 128], ht[i][:], reads=[b_ht[i]], writes=[b_out])
    Pn.close()


BLK = 512


def n_moe_blocks(T):
    return (4 * T + 32 * (BLK - 1) + BLK - 1) // BLK


def phase2_const_inputs(nc, T):
    NBLK = n_moe_blocks(T)
    d = {}
    for nm, shp, dt in (("ident", [128, 128], F32), ("trius", [128, 128], F32), ("blk512", [128, NBLK], F32),
                        ("wiota", [128, 16], F32), ("piota", [128, 1], F32),
                        ("xidx_a", [128, 8, T // 512], I32), ("xidx_b", [128, 8, T // 512], I32)):
        d[nm] = nc.dram_tensor("p_" + nm, shp, dt, kind="ExternalInput").ap()
    return d


def phase2_const_host(T, j):
    NBLK = n_moe_blocks(T)
    p = np.arange(128)
    s_ = p[:, None]; t_ = p[None, :]
    wi = np.zeros((128, 16), np.float32)
    for kc in range(16):
        wi[:, kc] = kc * 128 + p
    ntb = T // 512
    xi = np.zeros((128, 8, ntb), np.int32)
    for fc in range(8):
        for tb in range(ntb):
            xi[:, fc, tb] = ((fc * 128 + p) * 4 + j) * ntb + tb
    return {"p_ident": np.eye(128, dtype=np.float32), "p_trius": (s_ < t_).astype(np.float32),
            "p_blk512": np.tile((np.arange(NBLK) * 512.0)[None, :], (128, 1)).astype(np.float32),
            "p_wiota": wi, "p_piota": p[:, None].astype(np.float32),
            "p_xidx_a": xi, "p_xidx_b": xi.copy()}


def phase2(nc, kb, Scope, T, dr, cst, mod, b_mod, hs1, b_hs1, outs):
    KC = D // 128
    NT = T // 128
    NBLK = n_moe_blocks(T)
    NROWS = NBLK * BLK
    h1T = nc.dram_tensor("h1T", [D, T], BF16).ap()
    sgT = nc.dram_tensor("sgT", [2 * D, T], BF16).ap()
    m1T = nc.dram_tensor("m1T", [D, T], F32).ap()
    mT = nc.dram_tensor("mT", [D, T], BF16).ap()
    x1 = nc.dram_tensor("x1", [T, D], F32).ap()
    h2d = nc.dram_tensor("h2d", [T, D], BF16).ap()
    Xs = nc.dram_tensor("Xs", [NROWS, D], BF16).ap()
    Ys = nc.dram_tensor("Ys", [NROWS, D], BF16).ap()
    b_h1T, b_sgT, b_m1T, b_mT, b_x1, b_h2d, b_Xs, b_Ys = (Buf(n) for n in ("h1T", "sgT", "m1T", "mT", "x1", "h2d", "Xs", "Ys"))

    C = Scope()
    b_c = Buf("p2c")
    identf = C.sb("p_identf", [128, 128], F32); identb = C.sb("p_identb", [128, 128], BF16)
    kb.dma("sp", identf[:], cst["ident"], writes=[b_c])
    kb.op("dve", lambda e: e.tensor_copy(out=identb[:], in_=identf[:]), reads=[b_c], writes=[b_c])

    norm_to_fm(nc, kb, Scope, dr["xs"], T, hs1, b_hs1, mod, b_mod, identb, b_c, h1T, b_h1T, tag="s1")

    E2 = Scope()
    sgo = [E2.sb(f"sgo{i}", [128, 512], BF16) for i in range(2)]; b_sgo = [Buf("sgo0"), Buf("sgo1")]
    ec = [0]

    def epi2(n0, nw, t0, tw, ps, bps):
        i = ec[0] % 2; ec[0] += 1
        kb.op("act", lambda e: e.activation(out=sgo[i][:nw, :tw], in_=ps[:nw, :tw], func=AF.Sigmoid), reads=[bps], writes=[b_sgo[i]])
        kb.dma("sp", sgT[n0:n0 + nw, t0:t0 + tw], sgo[i][:nw, :tw], reads=[b_sgo[i]], writes=[b_sgT])
    b_h1T_r = b_h1T
    kb.barrier()
    gemm(nc, kb, Scope, dr["wgate"], D, 2 * D, h1T, T, "fm", epi2, tag="g2")
    E2.close()

    for which in (0, 1):
        E3 = Scope()
        sgi = [E3.sb(f"sgi{i}", [128, 512], BF16) for i in range(2)]; b_sgi = [Buf("sgi0"), Buf("sgi1")]
        m1i = [E3.sb(f"m1i{i}", [128, 512], F32) for i in range(2)]; b_m1i = [Buf("m1i0"), Buf("m1i1")]
        mo = [E3.sb(f"mo{i}", [128, 512], F32) for i in range(2)]; b_mo = [Buf("mo0"), Buf("mo1")]
        mob = [E3.sb(f"mob{i}", [128, 512], BF16) for i in range(2)]; b_mob = [Buf("mob0"), Buf("mob1")]
        xidx = E3.sb("xidx", [128, 8, T // 512], I32); b_xidx = Buf("xidx")
        kb.dma("sp", xidx[:], cst["xidx_a" if which == 0 else "xidx_b"], writes=[b_xidx])
        rows = dr["oa_rows" if which == 0 else "ob_rows"]
        ec3 = [0]

        def epi3(n0, nw, t0, tw, ps, bps, which=which):
            i = ec3[0] % 2; ec3[0] += 1
            kb.dma("act", sgi[i][:nw, :tw], sgT[which * D + n0:which * D + n0 + nw, t0:t0 + tw], reads=[b_sgT], writes=[b_sgi[i]])
            if which == 0:
                kb.op("dve", lambda e: e.tensor_tensor(out=mo[i][:nw, :tw], in0=ps[:nw, :tw], in1=sgi[i][:nw, :tw], op=ALU.mult),
                      reads=[bps, b_sgi[i]], writes=[b_mo[i]])
                kb.dma("sp", m1T[n0:n0 + nw, t0:t0 + tw], mo[i][:nw, :tw], reads=[b_mo[i]], writes=[b_m1T])
            else:
                kb.dma("act", m1i[i][:nw, :tw], m1T[n0:n0 + nw, t0:t0 + tw], reads=[b_m1T], writes=[b_m1i[i]])
                kb.op("dve", lambda e: e.tensor_tensor(out=mo[i][:nw, :tw], in0=ps[:nw, :tw], in1=sgi[i][:nw, :tw], op=ALU.mult),
                      reads=[bps, b_sgi[i]], writes=[b_mo[i]])
                kb.op("dve", lambda e: e.tensor_tensor(out=mob[i][:nw, :tw], in0=mo[i][:nw, :tw], in1=m1i[i][:nw, :tw], op=ALU.add),
                      reads=[b_mo[i], b_m1i[i]], writes=[b_mob[i]])
                kb.dma("sp", mT[n0:n0 + nw, t0:t0 + tw], mob[i][:nw, :tw], reads=[b_mob[i]], writes=[b_mT])

        def xload(t0, Xf, b_Xf, rows=rows):
            tb = t0 // 512
            for fc in range(8):
                kb.idma_gather(Xf[:, fc, :], rows, xidx[:, fc, tb:tb + 1], reads=[b_xidx], writes=[b_Xf])
        kb.barrier()
        gemm(nc, kb, Scope, dr["wba" if which == 0 else "wbb"], 1024, D, None, T, "fm", epi3, x_f32=True, tag=f"g3{which}", xload=xload)
        E3.close()

    E4 = Scope()
    g1bc = E4.sb("g1bc", [128, D], F32); b_g1bc = Buf("g1bc")
    kb.dma("sp", g1bc[:], dr["modn"][2:3, :].partition_broadcast(128), writes=[b_g1bc])
    xi4 = [E4.sb(f"xi4{i}", [128, 512], F32) for i in range(2)]; b_xi4 = [Buf("xi40"), Buf("xi41")]
    xo4 = [E4.sb(f"xo4{i}", [128, 512], F32) for i in range(2)]; b_xo4 = [Buf("xo40"), Buf("xo41")]
    ec4 = [0]

    def epi4(t0, tw, n0, ng, ps, bps):
        i = ec4[0] % 2; ec4[0] += 1
        kb.dma("act", xi4[i][:, :ng], dr["xs"][t0:t0 + 128, n0:n0 + ng], writes=[b_xi4[i]])
        kb.op("dve", lambda e: e.tensor_tensor(out=xo4[i][:, :ng], in0=ps[:, :ng], in1=g1bc[:, n0:n0 + ng], op=ALU.mult),
              reads=[bps, b_g1bc], writes=[b_xo4[i]])
        kb.op("pool", lambda e: e.tensor_tensor(out=xo4[i][:, :ng], in0=xo4[i][:, :ng], in1=xi4[i][:, :ng], op=ALU.add),
              reads=[b_xo4[i], b_xi4[i]], writes=[b_xo4[i]])
        kb.dma("sp", x1[t0:t0 + 128, n0:n0 + ng], xo4[i][:, :ng], reads=[b_xo4[i]], writes=[b_x1])
    kb.barrier()
    gemm(nc, kb, Scope, dr["wout"], D, D, mT, T, "tm", epi4, tag="g4")
    E4.close()
    kb.barrier()

    R = Scope()
    bexp = R.sb("bexp", [128, NBLK], F32); GK = R.sb("GK", [128, NT, 4], F32); DKI = R.sb("DKI", [128, NT, 4], I32)
    cnt_run = R.sb("cnt_run", [128, 32], F32); b_cnt = Buf("cnt")
    g2bc = R.sb("g2bc", [128, D], F32)
    R1 = Scope()
    LG = R1.sb("LG", [128, NT, 32], F32); T8 = R1.sb("T8", [128, NT, 8], F32)
    Mt = R1.sb("Mt", [128, NT, 32], F32); GW = R1.sb("GW", [128, NT, 32], F32); RK = R1.sb("RK", [128, NT, 32], F32)
    b_rt = Buf("route")
    kb.op("dve", lambda e: e.memset(cnt_run[:], 0.0), writes=[b_cnt])
    trius = R1.sb("trius", [128, 128], F32); onesf = R1.sb("p_onesf", [128, 128], F32)
    kb.dma("sp", trius[:], cst["trius"], writes=[b_c])
    kb.op("dve", lambda e: e.memset(onesf[:], 1.0), writes=[b_c])
    kb.dma("sp", g2bc[:], dr["modn"][5:6, :].partition_broadcast(128), writes=[b_c])
    N5 = Scope()
    hs2 = N5.sb("hs2", [128, D], F32); sh2 = N5.sb("sh2", [128, D], F32); n2g = N5.sb("n2g", [128, D], F32)
    b_n5 = Buf("n5c")
    kb.dma("sp", hs2[:], dr["modn"][4:5, :].partition_broadcast(128), writes=[b_n5])
    kb.dma("sp", sh2[:], dr["modn"][3:4, :].partition_broadcast(128), writes=[b_n5])
    kb.dma("sp", n2g[:], dr["n2g"].partition_broadcast(128), writes=[b_n5])
    kb.op("dve", lambda e: e.scalar_tensor_tensor(out=hs2[:], in0=hs2[:], scalar=1.0, in1=n2g[:], op0=ALU.add, op1=ALU.mult), reads=[b_n5], writes=[b_n5])
    wr = N5.sb("wr", [128, KC, 32], F32); brb = N5.sb("brb", [128, 32], F32)
    kb.dma("sp", wr[:], dr["wr"].rearrange("(kc p) e -> p kc e", p=128), writes=[b_n5])
    kb.dma("sp", brb[:], dr["br"].partition_broadcast(128), writes=[b_n5])
    xt = [N5.sb(f"n5xt{i}", [128, D], F32) for i in range(2)]; b_xt = [Buf("n5xt0"), Buf("n5xt1")]
    h2f = N5.sb("h2f", [128, D], F32); b_h2f = Buf("h2f")
    h2b = [N5.sb(f"h2b{i}", [128, D], BF16) for i in range(2)]; b_h2b = [Buf("h2b0"), Buf("h2b1")]
    junk = N5.sb("n5junk", [128, D], BF16); b_junk = Buf("n5junk")
    ssq = N5.sb("n5ssq", [128, 1], F32); b_ssq = Buf("n5ssq")
    h2T = N5.sb("h2T", [128, KC, 128], F32); b_h2T = Buf("h2T")
    ptf = [N5.ps(f"n5pt{i}", [128, 512], F32) for i in range(2)]; b_ptf = [Buf("n5pt0"), Buf("n5pt1")]
    plg = N5.ps("n5plg", [128, 128], F32); b_plg = Buf("n5plg")
    ex = N5.sb("n5ex", [128, 32], F32); b_ex = Buf("n5ex")
    sm = N5.sb("n5sm", [128, 1], F32); b_sm = Buf("n5sm")
    for t in range(NT):
        i = t % 2
        kb.dma("sp", xt[i][:], x1[t * 128:(t + 1) * 128, :], reads=[b_x1], writes=[b_xt[i]])
        kb.op("act", lambda e: e.activation(out=junk[:], in_=xt[i][:], func=AF.Square, accum_out=ssq[:]), reads=[b_xt[i]], writes=[b_junk, b_ssq])
        kb.op("act", lambda e: e.activation(out=ssq[:], in_=ssq[:], func=AF.Sqrt, scale=1.0 / D, bias=1e-6), reads=[b_ssq], writes=[b_ssq])
        kb.op("dve", lambda e: e.reciprocal(out=ssq[:], in_=ssq[:]), reads=[b_ssq], writes=[b_ssq])
        kb.op("dve", lambda e: e.scalar_tensor_tensor(out=h2f[:], in0=xt[i][:], scalar=ssq[:, 0:1], in1=hs2[:], op0=ALU.mult, op1=ALU.mult),
              reads=[b_xt[i], b_ssq, b_n5], writes=[b_h2f])
        kb.op("pool", lambda e: e.tensor_tensor(out=h2f[:], in0=h2f[:], in1=sh2[:], op=ALU.add), reads=[b_h2f, b_n5], writes=[b_h2f])
        kb.op("act", lambda e: e.activation(out=h2b[i][:], in_=h2f[:], func=AF.Identity), reads=[b_h2f], writes=[b_h2b[i]])
        kb.dma("sp", h2d[t * 128:(t + 1) * 128, :], h2b[i][:], reads=[b_h2b[i]], writes=[b_h2d])
        for q in range(KC // 4):
            pi = q % 2
            for u in range(4):
                k = q * 4 + u
                kb.op("pe", lambda e: e.transpose(out=ptf[pi][:, u * 128:(u + 1) * 128], in_=h2f[:, k * 128:(k + 1) * 128], identity=identf[:]),
                      reads=[b_h2f, b_c], writes=[b_ptf[pi]])
            kb.op("dve", lambda e: e.tensor_copy(out=h2T[:, q * 4:(q + 1) * 4, :], in_=ptf[pi][:, :].rearrange("p (u n) -> p u n", u=4)),
                  reads=[b_ptf[pi]], writes=[b_h2T])
        for k in range(KC):
            kb.op("pe", lambda e: e.matmul(plg[:, 0:32], lhsT=h2T[:, k, :], rhs=wr[:, k, :], start=(k == 0), stop=(k == KC - 1)),
                  reads=[b_h2T, b_n5], writes=[b_plg])
        kb.op("dve", lambda e: e.tensor_tensor(out=LG[:, t, :], in0=plg[:, 0:32], in1=brb[:], op=ALU.add), reads=[b_plg, b_n5], writes=[b_rt])
        kb.op("dve", lambda e: e.max(out=T8[:, t, :], in_=LG[:, t, :]), reads=[b_rt], writes=[b_rt])
        kb.op("dve", lambda e: e.tensor_scalar(out=Mt[:, t, :], in0=LG[:, t, :], scalar1=T8[:, t, 3:4], scalar2=None, op0=ALU.is_ge), reads=[b_rt], writes=[b_rt])
        kb.op("dve", lambda e: e.tensor_scalar(out=ex[:], in0=LG[:, t, :], scalar1=T8[:, t, 0:1], scalar2=None, op0=ALU.subtract), reads=[b_rt], writes=[b_ex])
        kb.op("act", lambda e: e.activation(out=ex[:], in_=ex[:], func=AF.Exp), reads=[b_ex], writes=[b_ex])
        kb.op("dve", lambda e: e.tensor_tensor(out=ex[:], in0=ex[:], in1=Mt[:, t, :], op=ALU.mult), reads=[b_ex, b_rt], writes=[b_ex])
        kb.op("dve", lambda e: e.tensor_reduce(out=sm[:], in_=ex[:], axis=AX.X, op=ALU.add), reads=[b_ex], writes=[b_sm])
        kb.op("dve", lambda e: e.reciprocal(out=sm[:], in_=sm[:]), reads=[b_sm], writes=[b_sm])
        kb.op("dve", lambda e: e.tensor_scalar(out=GW[:, t, :], in0=ex[:], scalar1=sm[:, 0:1], scalar2=None, op0=ALU.mult), reads=[b_ex, b_sm], writes=[b_rt])
        kb.op("pe", lambda e: e.matmul(plg[:, 32:64], lhsT=trius[:], rhs=Mt[:, t, :], start=True, stop=True), reads=[b_rt, b_c], writes=[b_plg])
        kb.op("pe", lambda e: e.matmul(plg[:, 64:96], lhsT=onesf[:], rhs=Mt[:, t, :], start=True, stop=True), reads=[b_rt, b_c], writes=[b_plg])
        kb.op("dve", lambda e: e.tensor_tensor(out=RK[:, t, :], in0=plg[:, 32:64], in1=cnt_run[:], op=ALU.add), reads=[b_plg, b_cnt], writes=[b_rt])
        kb.op("dve", lambda e: e.tensor_tensor(out=cnt_run[:], in0=plg[:, 64:96], in1=cnt_run[:], op=ALU.add), reads=[b_plg, b_cnt], writes=[b_cnt])
    N5.close()

    nbk = R1.sb("nbk", [128, 32], F32); tmpc = R1.sb("tmpc", [128, 32], F32); pend = R1.sb("pend", [128, 32], F32); pst = R1.sb("pst", [128, 32], F32)
    kb.op("dve", lambda e: e.memset(nbk[:], 0.0), writes=[b_rt])
    for m_ in range(T // BLK):
        kb.op("dve", lambda e: e.tensor_scalar(out=tmpc[:], in0=cnt_run[:], scalar1=float(BLK * m_), scalar2=None, op0=ALU.is_gt), reads=[b_cnt], writes=[b_rt])
        kb.op("dve", lambda e: e.tensor_tensor(out=nbk[:], in0=nbk[:], in1=tmpc[:], op=ALU.add), reads=[b_rt], writes=[b_rt])
    kb.op("dve", lambda e: e.tensor_scalar(out=nbk[:], in0=nbk[:], scalar1=float(BLK), scalar2=None, op0=ALU.mult), reads=[b_rt], writes=[b_rt])
    kb.op("dve", lambda e: e.tensor_copy(out=pend[:, 0:1], in_=nbk[:, 0:1]), reads=[b_rt], writes=[b_rt])
    for e_ in range(1, 32):
        kb.op("dve", lambda e: e.tensor_tensor(out=pend[:, e_:e_ + 1], in0=pend[:, e_ - 1:e_], in1=nbk[:, e_:e_ + 1], op=ALU.add), reads=[b_rt], writes=[b_rt])
    kb.op("dve", lambda e: e.tensor_tensor(out=pst[:], in0=pend[:], in1=nbk[:], op=ALU.subtract), reads=[b_rt], writes=[b_rt])
    blk512 = R1.sb("blk512", [128, NBLK], F32); tmpb = R1.sb("tmpb", [128, NBLK], F32)
    kb.dma("sp", blk512[:], cst["blk512"], writes=[b_c])
    kb.op("dve", lambda e: e.memset(bexp[:], 0.0), writes=[b_rt])
    for e_ in range(32):
        kb.op("dve", lambda e: e.tensor_scalar(out=tmpb[:], in0=blk512[:], scalar1=pend[:, e_:e_ + 1], scalar2=None, op0=ALU.is_ge), reads=[b_rt, b_c], writes=[b_rt])
        kb.op("dve", lambda e: e.tensor_tensor(out=bexp[:], in0=bexp[:], in1=tmpb[:], op=ALU.add), reads=[b_rt], writes=[b_rt])
    kb.op("dve", lambda e: e.tensor_scalar(out=bexp[:], in0=bexp[:], scalar1=31.0, scalar2=None, op0=ALU.min), reads=[b_rt], writes=[b_rt])
    kb.op("dve", lambda e: e.tensor_tensor(out=RK[:], in0=RK[:], in1=pst[:].unsqueeze(1).to_broadcast([128, NT, 32]), op=ALU.add), reads=[b_rt], writes=[b_rt])
    DK = R1.sb("DK", [128, NT, 4], F32)
    oh = R1.sb("oh", [128, NT, 32], F32); ohd = R1.sb("ohd", [128, NT, 32], F32)
    for k in range(4):
        kb.op("dve", lambda e: e.tensor_tensor(out=oh[:], in0=LG[:], in1=T8[:, :, k:k + 1].to_broadcast([128, NT, 32]), op=ALU.is_equal), reads=[b_rt], writes=[b_rt])
        kb.op("dve", lambda e: e.tensor_tensor(out=ohd[:], in0=oh[:], in1=RK[:], op=ALU.mult), reads=[b_rt], writes=[b_rt])
        kb.op("dve", lambda e: e.tensor_reduce(out=DK[:, :, k], in_=ohd[:], axis=AX.X, op=ALU.add), reads=[b_rt], writes=[b_rt])
        kb.op("dve", lambda e: e.tensor_tensor(out=ohd[:], in0=oh[:], in1=GW[:], op=ALU.mult), reads=[b_rt], writes=[b_rt])
        kb.op("dve", lambda e: e.tensor_reduce(out=GK[:, :, k], in_=ohd[:], axis=AX.X, op=ALU.add), reads=[b_rt], writes=[b_rt])
    kb.op("dve", lambda e: e.tensor_copy(out=DKI[:], in_=DK[:]), reads=[b_rt], writes=[b_rt])
    R1.close()

    DS = Scope()
    hr = [DS.sb(f"hr{i}", [128, D], BF16) for i in range(2)]; b_hr = [Buf("hr0"), Buf("hr1")]
    for t in range(NT):
        i = t % 2
        kb.dma("sp", hr[i][:], h2d[t * 128:(t + 1) * 128, :], reads=[b_h2d], writes=[b_hr[i]])
        for k in range(4):
            kb.idma_scatter(Xs, DKI[:, t, k:k + 1], hr[i][:], reads=[b_hr[i], b_rt], writes=[b_Xs])
    DS.close()

    MB = Scope()
    wiota = MB.sb("wiota", [128, 16], F32); piota = MB.sb("piota", [128, 1], F32)
    kb.dma("sp", wiota[:], cst["wiota"], writes=[b_c])
    kb.dma("sp", piota[:], cst["piota"], writes=[b_c])
    widf = MB.sb("widf", [128, 16], F32); widx = [MB.sb(f"widx{i}", [128, 16], I32) for i in range(2)]; b_widx = [Buf("widx0"), Buf("widx1")]
    bidf = MB.sb("bidf", [128, 2], F32); bidx = [MB.sb(f"bidx{i}", [128, 2], I32) for i in range(2)]
    Wf = [MB.sb(f"mWf{i}", [128, KC, 512], F32) for i in range(2)]; b_Wf = [Buf("mWf0"), Buf("mWf1")]
    Wb = [MB.sb(f"mWb{i}", [128, KC, 512], BF16) for i in range(2)]; b_Wb = [Buf("mWb0"), Buf("mWb1")]
    Xr = MB.sb("Xr", [128, 4, D], BF16); b_Xr = Buf("Xr")
    XT = MB.sb("XT", [128, KC, BLK], BF16); b_XT = Buf("XT")
    actT = MB.sb("actT", [128, KC, BLK], BF16); b_actT = Buf("actT")
    bgu = [MB.sb(f"bgu{i}", [128, 32], F32) for i in range(2)]; b_bgu = [Buf("bgu0"), Buf("bgu1")]
    bdb = [MB.sb(f"bdb{i}", [128, D], F32) for i in range(2)]; b_bdb = [Buf("bdb0"), Buf("bdb1")]
    gsb = MB.sb("gsb", [128, 512], F32); b_gsb = Buf("gsb")
    ssb = MB.sb("ssb", [128, 512], F32); b_ssb = Buf("ssb")
    usb = MB.sb("usb", [128, 512], F32); b_usb = Buf("usb")
    ysb = [MB.sb(f"ysb{i}", [128, 512], BF16) for i in range(2)]; b_ysb = [Buf("ysb0"), Buf("ysb1")]
    ptb = [MB.ps(f"mptb{i}", [128, 4, 128], BF16) for i in range(2)]; b_ptb = [Buf("mptb0"), Buf("mptb1")]
    pg = [MB.ps(f"mpg{i}", [128, 512], F32) for i in range(2)]; b_pg = [Buf("mpg0"), Buf("mpg1")]
    pu = [MB.ps(f"mpu{i}", [128, 512], F32) for i in range(2)]; b_pu = [Buf("mpu0"), Buf("mpu1")]
    py = [MB.ps(f"mpy{i}", [128, 512], F32) for i in range(2)]; b_py = [Buf("mpy0"), Buf("mpy1")]
    wct = [0]
    b_widf = Buf("widf")

    def load_w(rows_ap, wi_tile, b_wi, cg):
        i = wct[0] % 2; wct[0] += 1
        for kc in range(KC):
            kb.idma_gather(Wf[i][:, kc, :], rows_ap[cg], wi_tile[:, kc:kc + 1], reads=[b_wi], writes=[b_Wf[i]])
        h = KC // 2
        kb.op("act", lambda e: e.activation(out=Wb[i][:, :h, :], in_=Wf[i][:, :h, :], func=AF.Identity), reads=[b_Wf[i]], writes=[b_Wb[i]])
        kb.op("dve", lambda e: e.tensor_copy(out=Wb[i][:, h:, :], in_=Wf[i][:, h:, :]), reads=[b_Wf[i]], writes=[b_Wb[i]])
        return Wb[i], b_Wb[i]

    yc = 0
    for blk in range(NBLK):
        bi = blk % 2
        kb.op("dve", lambda e: e.scalar_tensor_tensor(out=widf[:], in0=bexp[:, blk:blk + 1].to_broadcast([128, 16]), scalar=2048.0, in1=wiota[:],
                                                      op0=ALU.mult, op1=ALU.add), reads=[b_rt, b_c], writes=[b_widf])
        kb.op("dve", lambda e: e.tensor_copy(out=widx[bi][:], in_=widf[:]), reads=[b_widf], writes=[b_widx[bi]])
        kb.op("dve", lambda e: e.scalar_tensor_tensor(out=bidf[:, 0:1], in0=bexp[:, blk:blk + 1], scalar=128.0, in1=piota[:], op0=ALU.mult, op1=ALU.add),
              reads=[b_rt, b_c], writes=[b_widf])
        kb.op("dve", lambda e: e.tensor_copy(out=bidf[:, 1:2], in_=bexp[:, blk:blk + 1]), reads=[b_rt], writes=[b_widf])
        kb.op("dve", lambda e: e.tensor_copy(out=bidx[bi][:], in_=bidf[:]), reads=[b_widf], writes=[b_widx[bi]])
        kb.idma_gather(bgu[bi][:], dr["bgu_tab"], bidx[bi][:, 0:1], reads=[b_widx[bi]], writes=[b_bgu[bi]])
        kb.idma_gather(bdb[bi][:], dr["bd_tab"], bidx[bi][:, 1:2], reads=[b_widx[bi]], writes=[b_bdb[bi]])
        for s4 in range(4):
            kb.dma("sp", Xr[:, s4, :], Xs[blk * BLK + s4 * 128:blk * BLK + (s4 + 1) * 128, :], reads=[b_Xs], writes=[b_Xr])
        for kc in range(KC):
            pi = kc % 2
            for s4 in range(4):
                kb.op("pe", lambda e: e.transpose(out=ptb[pi][:, s4, :], in_=Xr[:, s4, kc * 128:(kc + 1) * 128], identity=identb[:]),
                      reads=[b_Xr, b_c], writes=[b_ptb[pi]])
            kb.op("dve", lambda e: e.tensor_copy(out=XT[:, kc, :].rearrange("p (s n) -> p s n", s=4), in_=ptb[pi][:]), reads=[b_ptb[pi]], writes=[b_XT])
        for cg in range(4):
            Wg_, bWg = load_w(dr["wg_rows"], widx[bi], b_widx[bi], cg)
            Wu_, bWu = load_w(dr["wu_rows"], widx[bi], b_widx[bi], cg)
            for fb in range(4):
                fi = cg * 4 + fb
                pi = fi % 2
                for kc in range(KC):
                    kb.op("pe", lambda e: e.matmul(pg[pi][:, :], lhsT=Wg_[:, kc, fb * 128:(fb + 1) * 128], rhs=XT[:, kc, :], start=(kc == 0), stop=(kc == KC - 1)),
                          reads=[bWg, b_XT], writes=[b_pg[pi]])
                for kc in range(KC):
                    kb.op("pe", lambda e: e.matmul(pu[pi][:, :], lhsT=Wu_[:, kc, fb * 128:(fb + 1) * 128], rhs=XT[:, kc, :], start=(kc == 0), stop=(kc == KC - 1)),
                          reads=[bWu, b_XT], writes=[b_pu[pi]])
                kb.op("dve", lambda e: e.tensor_scalar(out=gsb[:], in0=pg[pi][:, :], scalar1=bgu[bi][:, fi:fi + 1], scalar2=7.0, op0=ALU.add, op1=ALU.min),
                      reads=[b_pg[pi], b_bgu[bi]], writes=[b_gsb])
                kb.op("act", lambda e: e.activation(out=ssb[:], in_=gsb[:], func=AF.Sigmoid, scale=1.702), reads=[b_gsb], writes=[b_ssb])
                kb.op("dve", lambda e: e.tensor_scalar(out=usb[:], in0=pu[pi][:, :], scalar1=bgu[bi][:, 16 + fi:17 + fi], scalar2=7.0, op0=ALU.add, op1=ALU.min),
                      reads=[b_pu[pi], b_bgu[bi]], writes=[b_usb])
                kb.op("pool", lambda e: e.tensor_scalar(out=usb[:], in0=usb[:], scalar1=-7.0, scalar2=1.0, op0=ALU.max, op1=ALU.add), reads=[b_usb], writes=[b_usb])
                kb.op("pool", lambda e: e.tensor_tensor(out=gsb[:], in0=gsb[:], in1=ssb[:], op=ALU.mult), reads=[b_gsb, b_ssb], writes=[b_gsb])
                kb.op("dve", lambda e: e.tensor_tensor(out=actT[:, fi, :], in0=gsb[:], in1=usb[:], op=ALU.mult), reads=[b_gsb, b_usb], writes=[b_actT])
        for cg in range(4):
            Wd_, bWd = load_w(dr["wd_rows"], widx[bi], b_widx[bi], cg)
            for s4 in range(4):
                pi = yc % 2; yc += 1
                for fc in range(KC):
                    kb.op("pe", lambda e: e.matmul(py[pi][:, :], lhsT=actT[:, fc, s4 * 128:(s4 + 1) * 128], rhs=Wd_[:, fc, :], start=(fc == 0), stop=(fc == KC - 1)),
                          reads=[bWd, b_actT], writes=[b_py[pi]])
                kb.op("dve", lambda e: e.tensor_tensor(out=ysb[pi][:], in0=py[pi][:, :], in1=bdb[bi][:, cg * 512:(cg + 1) * 512], op=ALU.add),
                      reads=[b_py[pi], b_bdb[bi]], writes=[b_ysb[pi]])
                kb.dma("sp", Ys[blk * BLK + s4 * 128:blk * BLK + (s4 + 1) * 128, cg * 512:(cg + 1) * 512], ysb[pi][:], reads=[b_ysb[pi]], writes=[b_Ys])
    MB.close()

    CB = Scope()
    yr = [CB.sb(f"yr{i}", [128, D], BF16) for i in range(2)]; b_yr = [Buf("yr0"), Buf("yr1")]
    acc = CB.sb("acc", [128, D], F32); b_acc = Buf("acc")
    xo = [CB.sb(f"cxo{i}", [128, D], F32) for i in range(2)]; b_xo = [Buf("cxo0"), Buf("cxo1")]
    yct = 0
    for t in range(NT):
        i = t % 2
        kb.dma("sp", xo[i][:], x1[t * 128:(t + 1) * 128, :], reads=[b_x1], writes=[b_xo[i]])
        for k in range(4):
            yi = yct % 2; yct += 1
            kb.idma_gather(yr[yi][:], Ys, DKI[:, t, k:k + 1], reads=[b_rt, b_Ys], writes=[b_yr[yi]])
            if k == 0:
                kb.op("dve", lambda e: e.tensor_scalar(out=acc[:], in0=yr[yi][:], scalar1=GK[:, t, 0:1], scalar2=None, op0=ALU.mult), reads=[b_yr[yi], b_rt], writes=[b_acc])
            else:
                kb.op("dve", lambda e: e.scalar_tensor_tensor(out=acc[:], in0=yr[yi][:], scalar=GK[:, t, k:k + 1], in1=acc[:], op0=ALU.mult, op1=ALU.add),
                      reads=[b_yr[yi], b_rt, b_acc], writes=[b_acc])
        kb.op("pool", lambda e: e.tensor_tensor(out=acc[:], in0=acc[:], in1=g2bc[:], op=ALU.mult), reads=[b_acc, b_c], writes=[b_acc])
        kb.op("dve", lambda e: e.tensor_tensor(out=xo[i][:], in0=xo[i][:], in1=acc[:], op=ALU.add), reads=[b_acc, b_xo[i]], writes=[b_xo[i]])
        ob = Buf("o"); outs.append(ob)
        kb.dma("sp", dr["out"][t * 128:(t + 1) * 128, :], xo[i][:], reads=[b_xo[i]], writes=[ob])
    CB.close()
    R.close()
    C.close()


def p2_dram_inputs(nc, T, S):
    d = {}
    ntb = T // 512
    for nm, shp in (("xs", [T, D]), ("n2g", [1, D]), ("wgate", [D, 2 * D]), ("wba", [1024, D]), ("wbb", [1024, D]), ("wout", [D, D]),
                    ("wr", [D, 32]), ("br", [1, 32]), ("bgu_tab", [4096, 32]), ("bd_tab", [32, D])):
        d[nm] = nc.dram_tensor("q_" + nm, shp, F32, kind="ExternalInput").ap()
    return d


def build_p2_test(T):
    S = 4 * T
    nc = bass.Bass("TRN2", target_bir_lowering=False)
    dr = p2_dram_inputs(nc, T, S)
    ntb = T // 512
    dr["modn"] = nc.dram_tensor("q_modn", [6, D], F32, kind="ExternalInput").ap()
    dr["oa_rows"] = nc.dram_tensor("q_oa_rows", [1024 * 4 * ntb, 512], F32, kind="ExternalInput").ap()
    dr["ob_rows"] = nc.dram_tensor("q_ob_rows", [1024 * 4 * ntb, 512], F32, kind="ExternalInput").ap()
    for nm in ("wg_rows", "wu_rows", "wd_rows"):
        dr[nm] = [nc.dram_tensor(f"q_{nm}{cg}", [32 * 2048, 512], F32, kind="ExternalInput").ap() for cg in range(4)]
    dr["out"] = nc.dram_tensor("out", [T, D], F32, kind="ExternalOutput").ap()
    modf = nc.dram_tensor("q_modf", [128, 96], F32, kind="ExternalInput").ap()
    hs1f = nc.dram_tensor("q_hs1", [128, 16], F32, kind="ExternalInput").ap()
    cst = phase2_const_inputs(nc, T)
    kb = KB(nc)
    Scope = make_scope(nc, kb)
    G = Scope()
    mod = G.sb("mod", [128, 96], F32); b_mod = Buf("mod")
    hs1 = G.sb("hs1", [128, 16], F32); b_hs1 = Buf("hs1")
    kb.dma("sp", mod[:], modf, writes=[b_mod])
    kb.dma("sp", hs1[:], hs1f, writes=[b_hs1])
    outs = []
    phase2(nc, kb, Scope, T, dr, cst, mod, b_mod, hs1, b_hs1, outs)
    kb.drain("sp", outs)
    return nc


G4 = [[0, 1, 2, 3], [4, 5, 6, 7]]
G8 = [list(range(8))]


def build_full(S, stage=99):
    T = S // 4
    KC = D // 128
    ntb = T // 512
    nc = bass.Bass("TRN2", target_bir_lowering=False)
    kb = KB(nc)
    Scope = make_scope(nc, kb)
    outs = []

    def ext(name, shape, dt=F32):
        return nc.dram_tensor(name, shape, dt, kind="ExternalInput").ap()

    def internal(name, shape, dt=F32):
        return nc.dram_tensor(name, shape, dt)

    xs = ext("xs", [T, D]); c_in = ext("c", [128, KC]); b_ada = ext("b_ada", [128, 6 * KC]); g1n = ext("norm1_gain", [128, KC])
    w_in1 = ext("w_in1", [D, 1984]); ident_in = ext("ident_in", [128, 128])
    sh_in = {"w_ada": ext("w_ada_sh", [512, 6 * D]), "wgate": ext("wgate_sh", [512, 2 * D]), "wba": ext("wba_sh", [256, D]),
             "wbb": ext("wbb_sh", [256, D]), "wout": ext("wout_sh", [512, D])}
    exp_in = {nm: ext(nm + "_sh", [4, 8 * D, 512]) for nm in ("wg", "wu", "wd")}
    dr = {"xs": xs, "n2g": ext("q_n2g", [1, D]), "wr": ext("q_wr", [D, 32]), "br": ext("q_br", [1, 32]),
          "bgu_tab": ext("q_bgu_tab", [4096, 32]), "bd_tab": ext("q_bd_tab", [32, D])}
    dr["out"] = nc.dram_tensor("out", [T, D], F32, kind="ExternalOutput").ap()
    rc = rwkv_const_inputs(nc)
    ac = attn_const_inputs(nc, S)
    pc = phase2_const_inputs(nc, T)

    def gather(name, src_ap, shape, groups, gsz, src_internal=False):
        rows, cols = shape
        R = max(1, min(rows, (1 << 20) // (4 * cols)))
        while rows % R:
            R -= 1
        big = internal(name + "_all", [gsz * rows, cols])
        bigv = big.ap().rearrange("(q r) c -> q r c", q=gsz)
        bb_big = Buf(name + "_all")
        NPAIR = 2
        bns = [internal(f"{name}_bn{i}", [R, cols]) for i in range(NPAIR)]
        ogs = [internal(f"{name}_og{i}", [gsz * R, cols]) for i in range(NPAIR)]
        b_bn = [Buf("bn") for _ in range(NPAIR)]; b_og = [Buf("og") for _ in range(NPAIR)]
        for k, r0 in enumerate(range(0, rows, R)):
            i = k % NPAIR
            kb.dma("act", bns[i].ap(), src_ap[r0:r0 + R, :], writes=[b_bn[i]])
            kb.collective("AllGather", groups, bns[i].ap().opt(), ogs[i].ap().opt(), reads=[b_bn[i]], writes=[b_og[i]])
            kb.dma("sp", bigv[:, r0:r0 + R, :], ogs[i].ap().rearrange("(q r) c -> q r c", q=gsz), reads=[b_og[i]], writes=[Buf("bigc")])
        kb.barrier()
        return big, bb_big

    xg_t, b_xg = gather("xg", xs, [T, D], G4, 4)
    wada_t, b_wada = gather("wada", sh_in["w_ada"], [512, 6 * D], G4, 4)
    wgate_t, b_wgate = gather("wgate", sh_in["wgate"], [512, 2 * D], G4, 4)
    wba_t, b_wba = gather("wba", sh_in["wba"], [256, D], G4, 4)
    wbb_t, b_wbb = gather("wbb", sh_in["wbb"], [256, D], G4, 4)
    wout_t, b_wout = gather("wout", sh_in["wout"], [512, D], G4, 4)
    exp_all = {}
    for nm in ("wg", "wu", "wd"):
        exp_all[nm] = []
        for cg in range(4):
            full, _ = gather(f"{nm}{cg}", exp_in[nm][cg], [8 * D, 512], G4, 4)
            exp_all[nm].append(full.ap())
    kb.barrier()
    xg = xg_t.ap(); w_ada = wada_t.ap()
    if stage == 0:
        ob = Buf("o"); outs.append(ob)
        kb.dma("sp", dr["out"], exp_all["wd"][3][31 * D:31 * D + T, :].rearrange("t (a c) -> t a c", a=1)[:, 0, :] if False else xg[T:2 * T, :], writes=[ob])
        kb.drain("sp", outs)
        return nc

    G = Scope()
    ident = G.sb("ident", [128, 128], BF16)
    identf = G.sb("identf", [128, 128], F32)
    b_ident = Buf("ident")
    kb.dma("sp", identf[:], ident_in[:, :], writes=[b_ident])
    kb.op("dve", lambda e: e.tensor_copy(out=ident[:], in_=identf[:]), reads=[b_ident], writes=[b_ident])
    mod = G.sb("mod", [128, 6 * KC], F32); b_mod = Buf("mod")
    hs = G.sb("hs", [128, KC], F32); b_hs = Buf("hs")
    modn_t = internal("modn", [6 * KC, 128]); b_modn = Buf("modn")

    A = Scope()
    c_sb = A.sb("c_sb", [128, KC], F32); b_c = Buf("c")
    bada = A.sb("bada", [128, 6 * KC], F32); b_bada = Buf("bada")
    kb.dma("sp", c_sb[:], c_in[:, :], writes=[b_c])
    kb.dma("sp", bada[:], b_ada[:, :], writes=[b_bada])
    kb.op("act", lambda e: e.activation(out=c_sb[:], in_=c_sb[:], func=AF.Silu), reads=[b_c], writes=[b_c])
    GW_ = 256
    wa_f = [A.sb(f"wa_f{i}", [128, KC, GW_], F32) for i in range(2)]
    b_waf = [Buf("waf0"), Buf("waf1")]
    pmod = A.ps("pmod", [128, 8]); b_pmod = Buf("pmod")
    w_ada_v = w_ada.rearrange("(kc p) n -> p kc n", p=128)
    for g in range(6 * D // GW_):
        i = g % 2
        kb.dma("sp" if i == 0 else "act", wa_f[i][:], w_ada_v[:, :, g * GW_:(g + 1) * GW_], writes=[b_waf[i]])
        for nb in range(GW_ // 128):
            col = g * (GW_ // 128) + nb
            pc_ = (col % 4) * 2
            for k in range(KC):
                kb.op("pe", lambda e: e.matmul(pmod[:, pc_:pc_ + 1], lhsT=wa_f[i][:, k, nb * 128:(nb + 1) * 128],
                                               rhs=c_sb[:, k:k + 1], start=(k == 0), stop=(k == KC - 1)),
                      reads=[b_waf[i], b_c], writes=[b_pmod])
            kb.op("dve", lambda e: e.tensor_tensor(out=mod[:, col:col + 1], in0=pmod[:, pc_:pc_ + 1], in1=bada[:, col:col + 1], op=ALU.add),
                  reads=[b_pmod, b_bada], writes=[b_mod])
    g1 = A.sb("g1", [128, KC], F32); b_g1 = Buf("g1")
    kb.dma("sp", g1[:], g1n[:, :], writes=[b_g1])
    kb.op("dve", lambda e: e.scalar_tensor_tensor(out=hs[:], in0=mod[:, KC:2 * KC], scalar=1.0, in1=g1[:], op0=ALU.add, op1=ALU.mult),
          reads=[b_mod, b_g1], writes=[b_hs])
    pmt = A.ps("pmt", [128, 128]); b_pmt = Buf("pmt")
    modT = A.sb("modT", [128, 128], F32); b_modT = Buf("modT")
    kb.op("pe", lambda e: e.transpose(out=pmt[:6 * KC, :], in_=mod[:, :], identity=identf[:]), reads=[b_mod, b_ident], writes=[b_pmt])
    kb.op("act", lambda e: e.activation(out=modT[:6 * KC, :], in_=pmt[:6 * KC, :], func=AF.Identity), reads=[b_pmt], writes=[b_modT])
    kb.dma("sp", modn_t.ap(), modT[:6 * KC, :], reads=[b_modT], writes=[b_modn])
    A.close()
    dr["modn"] = modn_t.ap().rearrange("(s kc) p -> s (kc p)", s=6)

    NP1 = 1984
    projT_t = internal("projT", [NP1, S]); projT = projT_t.ap()
    P = Scope()
    TG = min(1024, S)
    w_v = w_in1.rearrange("(kc p) n -> p kc n", p=128)
    Wb = P.sb("Wb", [128, KC, NP1], BF16); b_Wb = Buf("Wb")
    wf = [P.sb(f"wf{i}", [128, KC, 128], F32) for i in range(2)]; b_wf = [Buf("wf0"), Buf("wf1")]
    nblocks = [(n0, min(128, NP1 - n0)) for n0 in range(0, NP1, 128)]
    for bi, (n0, nw) in enumerate(nblocks):
        i = bi % 2
        kb.dma("act", wf[i][:, :, :nw], w_v[:, :, n0:n0 + nw], writes=[b_wf[i]])
        kb.op("pool", lambda e: e.tensor_copy(out=Wb[:, :, n0:n0 + nw], in_=wf[i][:, :, :nw]), reads=[b_wf[i]], writes=[b_Wb])
    hT = [P.sb(f"hT{i}", [128, KC, TG], BF16) for i in range(2)]; b_hT = [Buf("hT0"), Buf("hT1")]
    xt = [P.sb(f"xt{i}", [128, D], F32) for i in range(2)]; b_xt = [Buf("xt0"), Buf("xt1")]
    xsb = [P.sb(f"xs{i}", [128, D], BF16) for i in range(2)]; b_xs = [Buf("xs0"), Buf("xs1")]
    junk = P.sb("junk", [128, D], BF16); b_junk = Buf("junk")
    ssq = [P.sb(f"ssq{i}", [128, 1], F32) for i in range(2)]; b_ssq = [Buf("ssq0"), Buf("ssq1")]
    ptr = [P.ps(f"ptr{i}", [128, 4, 128], BF16) for i in range(2)]; b_ptr = [Buf("ptr0"), Buf("ptr1")]
    pp = [P.ps(f"pp{i}", [128, 512]) for i in range(2)]; b_pp = [Buf("pp0"), Buf("pp1")]
    ot = [P.sb(f"ot{i}", [128, 512], F32) for i in range(2)]; b_ot = [Buf("ot0"), Buf("ot1")]
    b_projT = Buf("projT")
    TB = min(512, TG)
    cnt = 0
    tcount = 0
    for gi, tg0 in enumerate(range(0, S, TG)):
        hi = gi % 2
        for tt in range(TG // 128):
            t = tg0 // 128 + tt
            i = tcount % 2
            tcount += 1
            kb.dma("sp", xt[i][:], xg[t * 128:(t + 1) * 128, :], reads=[b_xg], writes=[b_xt[i]])
            kb.op("act", lambda e: e.activation(out=junk[:], in_=xt[i][:], func=AF.Square, accum_out=ssq[i][:]), reads=[b_xt[i]], writes=[b_junk, b_ssq[i]])
            kb.op("act", lambda e: e.activation(out=ssq[i][:], in_=ssq[i][:], func=AF.Sqrt, scale=1.0 / D, bias=1e-6), reads=[b_ssq[i]], writes=[b_ssq[i]])
            kb.op("dve", lambda e: e.reciprocal(out=ssq[i][:], in_=ssq[i][:]), reads=[b_ssq[i]], writes=[b_ssq[i]])
            kb.op("dve", lambda e: e.tensor_scalar(out=xsb[i][:], in0=xt[i][:], scalar1=ssq[i][:, 0:1], scalar2=None, op0=ALU.mult),
                  reads=[b_xt[i], b_ssq[i]], writes=[b_xs[i]])
            for q in range(KC // 4):
                pi = q % 2
                for u in range(4):
                    k = q * 4 + u
                    kb.op("pe", lambda e: e.transpose(out=ptr[pi][:, u, :], in_=xsb[i][:, k * 128:(k + 1) * 128], identity=ident[:]),
                          reads=[b_xs[i], b_ident], writes=[b_ptr[pi]])
                for u in range(4):
                    k = q * 4 + u
                    kb.op("act", lambda e: e.activation(out=hT[hi][:, k, tt * 128:(tt + 1) * 128], in_=ptr[pi][:, u, :],
                                                        func=AF.Identity, scale=hs[:, k:k + 1], bias=mod[:, k:k + 1]),
                          reads=[b_ptr[pi], b_hs, b_mod], writes=[b_hT[hi]])
        for bi, (n0, nw) in enumerate(nblocks):
            for t0 in range(0, TG, TB):
                pi = cnt % 2
                cnt += 1
                for k in range(KC):
                    kb.op("pe", lambda e: e.matmul(pp[pi][:nw, :TB], lhsT=Wb[:, k, n0:n0 + nw], rhs=hT[hi][:, k, t0:t0 + TB],
                                                   start=(k == 0), stop=(k == KC - 1)),
                          reads=[b_Wb, b_hT[hi]], writes=[b_pp[pi]])
                kb.op("act" if pi == 0 else "dve",
                      (lambda e: e.activation(out=ot[pi][:nw, :TB], in_=pp[pi][:nw, :TB], func=AF.Identity)) if pi == 0 else
                      (lambda e: e.tensor_copy(out=ot[pi][:nw, :TB], in_=pp[pi][:nw, :TB])),
                      reads=[b_pp[pi]], writes=[b_ot[pi]])
                kb.dma("sp", projT[n0:n0 + nw, tg0 + t0:tg0 + t0 + TB], ot[pi][:nw, :TB], reads=[b_ot[pi]], writes=[Buf("pj")])
    P.close()

    oaT_t = internal("oaT", [256, S]); obT_t = internal("obT", [256, S])
    mo = []
    phase_rwkv(nc, kb, Scope, None, projT, S, rc, oaT_t.ap(), mo, fm_out=True)
    phase_attn(nc, kb, Scope, projT, S, ac, obT_t.ap(), mo, row0=1216)
    kb.barrier()
    oa_all, _ = gather("oaT", oaT_t.ap(), [256, S], G4, 4)
    ob_all, _ = gather("obT", obT_t.ap(), [256, S], G4, 4)
    kb.barrier()

    dr["oa_rows"] = oa_all.ap().rearrange("f (r c) -> (f r) c", c=512)
    dr["ob_rows"] = ob_all.ap().rearrange("f (r c) -> (f r) c", c=512)
    dr["wgate"] = wgate_t.ap(); dr["wba"] = wba_t.ap(); dr["wbb"] = wbb_t.ap(); dr["wout"] = wout_t.ap()
    dr["wg_rows"] = exp_all["wg"]; dr["wu_rows"] = exp_all["wu"]; dr["wd_rows"] = exp_all["wd"]
    phase2(nc, kb, Scope, T, dr, pc, mod, b_mod, hs, b_hs, outs)
    G.close()
    kb.drain("sp", outs)
    return nc


def make_in_maps(inp, S):
    T = S // 4
    f = np.float32
    g = lambda k: np.asarray(inp[k])[0]
    x = np.asarray(inp["x"]); c = np.asarray(inp["c"])
    w_in = g("w_in"); w_ada = g("w_ada")
    bg, bu, bd = g("b_exp_gate"), g("b_exp_up"), g("b_exp_down")
    bgu_tab = np.ascontiguousarray(np.concatenate([bg.reshape(32, 16, 128).transpose(0, 2, 1), bu.reshape(32, 16, 128).transpose(0, 2, 1)], axis=2).reshape(4096, 32))
    rch = rwkv_const_host(); ach = attn_const_host(S)
    blocks = [(0, 128), (128, 128), (256, 128), (384, 128), (512, 128), (640, 128), (768, 96), (864, 96), (960, 128), (1088, 128)]
    maps = []
    for i in range(NCORES):
        b, j = i // 4, i % 4
        cols = core_cols(j)
        hc = slice(256 * j, 256 * (j + 1))
        mu_l = g("shift_mu")[cols[:1216]]
        mub = np.zeros((128, 10), f)
        for q, (r0, nr) in enumerate(blocks):
            mub[:nr, q] = mu_l[r0:r0 + nr]
        m = {
            "xs": np.ascontiguousarray(x[b, j * T:(j + 1) * T]), "c": _feat(c[b]),
            "b_ada": np.ascontiguousarray(g("b_ada").reshape(96, 128).T), "norm1_gain": _feat(g("norm1_gain")),
            "w_in1": np.ascontiguousarray(w_in[:, cols]), "ident_in": np.eye(128, dtype=f),
            "w_ada_sh": np.ascontiguousarray(w_ada[512 * j:512 * (j + 1)]),
            "wgate_sh": np.ascontiguousarray(w_in[512 * j:512 * (j + 1), 6592:10688]),
            "wba_sh": np.ascontiguousarray(g("w_branch_a")[256 * j:256 * (j + 1)]),
            "wbb_sh": np.ascontiguousarray(g("w_branch_b")[256 * j:256 * (j + 1)]),
            "wout_sh": np.ascontiguousarray(g("w_out")[512 * j:512 * (j + 1)]),
            "q_n2g": g("norm2_gain")[None, :], "q_wr": g("w_router"), "q_br": g("b_router")[None, :],
            "q_bgu_tab": bgu_tab, "q_bd_tab": np.ascontiguousarray(bd),
            "r_vecs": np.stack([g(k)[hc] for k in ("w0", "a0", "k_k", "k_a", "r_k", "gn_w", "gn_b")]).astype(f), "r_mu": mub,
            "r_wdu": np.ascontiguousarray(g("w_decay_up")[:, hc]), "r_wau": np.ascontiguousarray(g("w_aaa_up")[:, hc]),
            "r_wgu": np.ascontiguousarray(g("w_gate_up")[:, hc]),
            "a_gq": np.tile(g("q_gain"), 2)[:, None].astype(f), "a_gk": np.tile(g("k_gain"), 2)[:, None].astype(f),
            "a_gs": g("subln_gain")[:, None].astype(f),
            "a_lam": np.concatenate([g("lam_q1"), g("lam_k1"), g("lam_q2"), g("lam_k2")])[None, :].astype(f),
        }
        for nm, key in (("wg", "w_exp_gate"), ("wu", "w_exp_up"), ("wd", "w_exp_down")):
            w = g(key)
            m[nm + "_sh"] = np.ascontiguousarray(np.stack([w[j * 8 + c8] for c8 in range(8)]).reshape(8, D, 4, 512).transpose(2, 0, 1, 3).reshape(4, 8 * D, 512))
        m.update(rch); m.update(ach); m.update(phase2_const_host(T, j))
        maps.append(m)
    return maps


_NC_CACHE = {}


def kernel(**inp):
    S = np.asarray(inp["x"]).shape[1]
    T = S // 4
    if S not in _NC_CACHE:
        _NC_CACHE[S] = build_full(S)
    nc = _NC_CACHE[S]
    maps = make_in_maps(inp, S)
    res = run_bass_kernel_spmd(nc, maps, core_ids=list(range(NCORES)))
    out = np.zeros((2, S, D), np.float32)
    for i in range(NCORES):
        b, j = i // 4, i % 4
        out[b, j * T:(j + 1) * T] = res.results[i]["out"]
    return out
```
